# Optimizing a Trainium2 kernel written in Bass

```python
import jax, jax.numpy as jnp
from jax import lax
import numpy as np

D_MODEL = 1024
BATCH = 4
SEQ = 8192
DEPTH = 2

HEAD_DIM = 64
GROUP_WIDTH = D_MODEL // 2
GROUP_HEADS = GROUP_WIDTH // HEAD_DIM
RW_HEADS = GROUP_HEADS
RW_DIM = GROUP_WIDTH
DECAY_LORA = 64
AAA_LORA = 64
GATE_LORA = 128
RW_GN_EPS = 64e-5
MLA_HEADS = GROUP_HEADS
MLA_NOPE = HEAD_DIM
MLA_ROPE = HEAD_DIM // 2
MLA_V = HEAD_DIM
MLA_Q_RANK = D_MODEL // 4
MLA_KV_RANK = D_MODEL // 8
FOX_HEADS = GROUP_HEADS
FOX_DIM = GROUP_WIDTH
MOBA_HEADS = GROUP_HEADS
MOBA_DIM = GROUP_WIDTH
MOBA_BLOCK = 256
MOBA_TOPK = 3
MOBA_Q_CHUNK = 32
Q_BLOCK = 128
ROPE_THETA = 10000.0
NORM_EPS = 1e-6
D_FF = 256 * ((8 * D_MODEL // 3 + 255) // 256)
N_EXPERTS = 8
TOP_K = 2
D_EXPERT = 7 * D_MODEL // 2

RW_SIZES = (RW_DIM, RW_DIM, RW_DIM, DECAY_LORA, AAA_LORA, GATE_LORA)
RW_COLS = sum(RW_SIZES)
MLA_SIZES = (MLA_Q_RANK, MLA_KV_RANK, MLA_ROPE)
IN0_COLS = RW_COLS + sum(MLA_SIZES)
FOX_SIZES = (FOX_DIM, FOX_DIM, FOX_DIM, FOX_HEADS)
FOX_COLS = sum(FOX_SIZES)
MOBA_SIZES = (MOBA_DIM, MOBA_DIM, MOBA_DIM)
IN1_COLS = FOX_COLS + sum(MOBA_SIZES)
MIX0_OUT = RW_DIM + MLA_HEADS * MLA_V
MIX1_OUT = FOX_DIM + MOBA_DIM

kernel_name = 'hybrid_rwkv7_mla_fox_moba_moe'


def split_cols(t, sizes):
    return jnp.split(t, [int(i) for i in np.cumsum(sizes)[:-1]], axis=-1)


def to_heads(t, n_heads):
    b, s, _ = t.shape
    return t.reshape(b, s, n_heads, -1)


def rmsnorm(x, g, eps=NORM_EPS):
    xf = x.astype(jnp.float32)
    xf = xf * lax.rsqrt(jnp.mean(xf * xf, axis=-1, keepdims=True) + eps)
    return (xf * g.astype(jnp.float32)).astype(x.dtype)


def rope_tables(seq_len, dim):
    inv_freq = ROPE_THETA ** (-jnp.arange(0, dim, 2, dtype=jnp.float32) / dim)
    ang = jnp.arange(seq_len, dtype=jnp.float32)[:, None] * inv_freq[None, :]
    return jnp.cos(ang), jnp.sin(ang)


def apply_rope(x, cos_r, sin_r):
    xf = x.astype(jnp.float32)
    x1, x2 = jnp.split(xf, 2, axis=-1)
    c, s = cos_r[:, None, :], sin_r[:, None, :]
    return jnp.concatenate([x1 * c - x2 * s, x2 * c + x1 * s], axis=-1).astype(x.dtype)


def swiglu(t, w_gate, w_up, w_down):
    return (jax.nn.silu(t @ w_gate) * (t @ w_up)) @ w_down


def block_causal_attention(q, k, v, scale, log_decay_cum=None):
    b, h, s, _ = q.shape
    k_pos = jnp.arange(s)

    def one_block(i):
        start = i * Q_BLOCK
        qb = lax.dynamic_slice_in_dim(q, start, Q_BLOCK, axis=2)
        logits = jnp.einsum('bhqd,bhkd->bhqk', qb, k).astype(jnp.float32) * scale
        q_pos = start + jnp.arange(Q_BLOCK)
        if log_decay_cum is not None:
            dq = lax.dynamic_slice_in_dim(log_decay_cum, start, Q_BLOCK, axis=2)
            logits = logits + dq[..., :, None] - log_decay_cum[..., None, :]
        logits = jnp.where(k_pos[None, :] <= q_pos[:, None], logits, -jnp.inf)
        p = jax.nn.softmax(logits, axis=-1).astype(v.dtype)
        return jnp.einsum('bhqk,bhkd->bhqd', p, v)

    out = lax.map(one_block, jnp.arange(s // Q_BLOCK))
    return out.transpose(1, 2, 0, 3, 4).reshape(b, h, s, -1)


def rwkv7_time_mix(r, k, v, w_down, a_down, g_down, w0, w2, a0, a2, g2,
                   k_k, k_a, r_k, lnx_g, lnx_b):
    out_dtype = r.dtype
    f32 = jnp.float32
    r, k, v, w_down, a_down, g_down = (t.astype(f32) for t in (r, k, v, w_down, a_down, g_down))
    b, s, c = r.shape
    h, n = RW_HEADS, HEAD_DIM
    log_w = -jax.nn.softplus(-(w0 + jnp.tanh(w_down) @ w2)) - 0.5
    decay = jnp.exp(-jnp.exp(log_w))
    a = jax.nn.sigmoid(a0 + a_down @ a2)
    g = jax.nn.sigmoid(g_down) @ g2
    kk = (k * k_k).reshape(b, s, h, n)
    kk = kk / jnp.maximum(jnp.linalg.norm(kk, axis=-1, keepdims=True), 1e-12)
    k = k * (1.0 + (a - 1.0) * k_a)
    r_h, w_h, k_h, v_h, a_h = (t.reshape(b, s, h, n) for t in (r, decay, k, v, a))
    b_h = kk * a_h
    xs = tuple(jnp.moveaxis(t, 1, 0) for t in (r_h, w_h, k_h, v_h, kk, b_h))

    def step(state, inp):
        r_t, w_t, k_t, v_t, kk_t, b_t = inp
        sa = jnp.einsum('bhvk,bhk->bhv', state, -kk_t)
        state = (state * w_t[:, :, None, :] + sa[..., None] * b_t[:, :, None, :]
                 + v_t[..., None] * k_t[:, :, None, :])
        return state, jnp.einsum('bhvk,bhk->bhv', state, r_t)

    state0 = jnp.zeros((b, h, n, n), f32)
    _, y = lax.scan(step, state0, xs)
    y = jnp.moveaxis(y, 0, 1)
    mu = jnp.mean(y, axis=-1, keepdims=True)
    var = jnp.mean(jnp.square(y - mu), axis=-1, keepdims=True)
    y = ((y - mu) * lax.rsqrt(var + RW_GN_EPS)).reshape(b, s, c) * lnx_g + lnx_b
    bonus = jnp.sum(r_h * k_h * r_k, axis=-1, keepdims=True) * v_h
    y = (y + bonus.reshape(b, s, c)) * g
    return y.astype(out_dtype)


def mla_attention(q_lat, kv_lat, k_rope, q_norm, w_uq, kv_norm, w_ukv, cos_r, sin_r):
    b, s, _ = q_lat.shape
    h = MLA_HEADS
    q = to_heads(rmsnorm(q_lat, q_norm) @ w_uq, h)
    q_nope, q_pe = q[..., :MLA_NOPE], apply_rope(q[..., MLA_NOPE:], cos_r, sin_r)
    kv = to_heads(rmsnorm(kv_lat, kv_norm) @ w_ukv, h)
    k_nope, v = kv[..., :MLA_NOPE], kv[..., MLA_NOPE:]
    k_pe = apply_rope(k_rope[:, :, None, :], cos_r, sin_r)
    q = jnp.concatenate([q_nope, q_pe], axis=-1)
    k = jnp.concatenate([k_nope, jnp.broadcast_to(k_pe, (b, s, h, MLA_ROPE))], axis=-1)
    out = block_causal_attention(q.transpose(0, 2, 1, 3), k.transpose(0, 2, 1, 3),
                                 v.transpose(0, 2, 1, 3), (MLA_NOPE + MLA_ROPE) ** -0.5)
    return out.transpose(0, 2, 1, 3).reshape(b, s, h * MLA_V)


def forgetting_attention(q, k, v, f_logit, b_f):
    b, s, _ = q.shape
    h = FOX_HEADS
    log_f = jax.nn.log_sigmoid(f_logit.astype(jnp.float32) + b_f.astype(jnp.float32))
    d_cum = jnp.cumsum(log_f, axis=1).transpose(0, 2, 1)
    qh, kh, vh = (to_heads(t, h).transpose(0, 2, 1, 3) for t in (q, k, v))
    out = block_causal_attention(qh, kh, vh, HEAD_DIM ** -0.5, d_cum)
    return out.transpose(0, 2, 1, 3).reshape(b, s, h * HEAD_DIM)


def moba_attention(q, k, v, cos_r, sin_r):
    b, s, _ = q.shape
    h, d = MOBA_HEADS, HEAD_DIM
    qh = apply_rope(to_heads(q, h), cos_r, sin_r).transpose(0, 2, 1, 3)
    kh = apply_rope(to_heads(k, h), cos_r, sin_r).transpose(0, 2, 1, 3)
    vh = to_heads(v, h).transpose(0, 2, 1, 3)
    nb = -(-s // MOBA_BLOCK)
    pad = nb * MOBA_BLOCK - s
    kp = jnp.pad(kh, ((0, 0), (0, 0), (0, pad), (0, 0)))
    vp = jnp.pad(vh, ((0, 0), (0, 0), (0, pad), (0, 0)))
    k_blocks = kp.reshape(b, h, nb, MOBA_BLOCK, d)
    v_blocks = vp.reshape(b, h, nb, MOBA_BLOCK, d)
    k_mean = jnp.mean(k_blocks.astype(jnp.float32), axis=3).astype(kh.dtype)
    n_sel = min(MOBA_TOPK, nb)
    n_sel_keys = n_sel * MOBA_BLOCK
    scale = d ** -0.5
    b_idx = jnp.arange(b)[:, None, None, None]
    h_idx = jnp.arange(h)[None, :, None, None]
    blk_ids = jnp.arange(nb)
    offs = jnp.arange(MOBA_BLOCK)

    def one_chunk(i):
        start = i * MOBA_Q_CHUNK
        qc = lax.dynamic_slice_in_dim(qh, start, MOBA_Q_CHUNK, axis=2)
        q_pos = start + jnp.arange(MOBA_Q_CHUNK)
        own = start // MOBA_BLOCK
        gate = jnp.einsum('bhqd,bhnd->bhqn', qc, k_mean).astype(jnp.float32)
        gate = jnp.where(blk_ids < own, gate, -jnp.inf)
        _, sel = lax.top_k(gate, n_sel)
        sel_valid = sel < own
        k_sel = k_blocks[b_idx, h_idx, sel]
        v_sel = v_blocks[b_idx, h_idx, sel]
        s_sel = jnp.einsum('bhqd,bhqjld->bhqjl', qc, k_sel).astype(jnp.float32) * scale
        s_sel = jnp.where(sel_valid[..., None], s_sel, -jnp.inf)
        k_own = lax.dynamic_slice_in_dim(kp, own * MOBA_BLOCK, MOBA_BLOCK, axis=2)
        v_own = lax.dynamic_slice_in_dim(vp, own * MOBA_BLOCK, MOBA_BLOCK, axis=2)
        s_own = jnp.einsum('bhqd,bhld->bhql', qc, k_own).astype(jnp.float32) * scale
        s_own = jnp.where((own * MOBA_BLOCK + offs)[None, :] <= q_pos[:, None], s_own, -jnp.inf)
        logits = jnp.concatenate([s_sel.reshape(b, h, MOBA_Q_CHUNK, n_sel_keys), s_own], axis=-1)
        p = jax.nn.softmax(logits, axis=-1).astype(vh.dtype)
        p_sel = p[..., :n_sel_keys].reshape(b, h, MOBA_Q_CHUNK, n_sel, MOBA_BLOCK)
        p_own = p[..., n_sel_keys:]
        return (jnp.einsum('bhqjl,bhqjld->bhqd', p_sel, v_sel)
                + jnp.einsum('bhql,bhld->bhqd', p_own, v_own))

    out = lax.map(one_chunk, jnp.arange(s // MOBA_Q_CHUNK))
    out = out.transpose(1, 2, 0, 3, 4).reshape(b, h, s, d)
    return out.transpose(0, 2, 1, 3).reshape(b, s, h * d)


def moe_swiglu(t, router, w_gate, w_up, w_down):
    logits = (t @ router).astype(jnp.float32)
    top_logits, top_idx = lax.top_k(logits, TOP_K)
    gates = jax.nn.softmax(top_logits, axis=-1)
    combine = jnp.sum(jax.nn.one_hot(top_idx, N_EXPERTS, dtype=jnp.float32) * gates[..., None], axis=1)
    out = jnp.zeros_like(t)
    for e in range(N_EXPERTS):
        out = out + combine[:, e:e + 1].astype(t.dtype) * swiglu(t, w_gate[e], w_up[e], w_down[e])
    return out


def even_layer(h, norm_mix, w_in, shift_mu, rw_w0, rw_w2, rw_a0, rw_a2, rw_g2, rw_kk, rw_ka,
               rw_rk, rw_lnx_g, rw_lnx_b, mla_qnorm, mla_wuq, mla_kvnorm, mla_wukv, w_out,
               norm_ffn, ffn_wg, ffn_wu, ffn_wd, cos_r, sin_r):
    u = rmsnorm(h, norm_mix)
    proj = u @ w_in
    rw_cols, mla_cols = proj[..., :RW_COLS], proj[..., RW_COLS:]
    rw_prev = jnp.pad(rw_cols[:, :-1], ((0, 0), (1, 0), (0, 0)))
    rw_cols = rw_cols + shift_mu * (rw_prev - rw_cols)
    y_a = rwkv7_time_mix(*split_cols(rw_cols, RW_SIZES), rw_w0, rw_w2, rw_a0, rw_a2, rw_g2,
                         rw_kk, rw_ka, rw_rk, rw_lnx_g, rw_lnx_b)
    y_b = mla_attention(*split_cols(mla_cols, MLA_SIZES), mla_qnorm, mla_wuq, mla_kvnorm,
                        mla_wukv, cos_r, sin_r)
    h = h + jnp.concatenate([y_a, y_b], axis=-1) @ w_out
    return h + swiglu(rmsnorm(h, norm_ffn), ffn_wg, ffn_wu, ffn_wd)


def odd_layer(h, norm_mix, w_in, fox_bf, w_out, norm_ffn, router, moe_wg, moe_wu, moe_wd,
              cos_r, sin_r):
    b, s, d = h.shape
    u = rmsnorm(h, norm_mix)
    proj = u @ w_in
    fox_cols, moba_cols = proj[..., :FOX_COLS], proj[..., FOX_COLS:]
    y_c = forgetting_attention(*split_cols(fox_cols, FOX_SIZES), fox_bf)
    y_d = moba_attention(*split_cols(moba_cols, MOBA_SIZES), cos_r, sin_r)
    h = h + jnp.concatenate([y_c, y_d], axis=-1) @ w_out
    t = rmsnorm(h, norm_ffn).reshape(b * s, d)
    return h + moe_swiglu(t, router, moe_wg, moe_wu, moe_wd).reshape(b, s, d)


def setup_inputs(seed: int = 0) -> dict:
    key = jax.random.key(seed)
    ks = iter(jax.random.split(key, 40))
    D = D_MODEL

    def nrm(shape, scale):
        return scale * jax.random.normal(next(ks), shape, jnp.float32)

    def gain(n):
        return 1.0 + nrm((n,), 0.05)

    return {
        'x': nrm((BATCH, SEQ, D), 1.0),
        'norm_mix_0': gain(D),
        'w_in_0': nrm((D, IN0_COLS), D ** -0.5),
        'shift_mu_0': jax.random.uniform(next(ks), (RW_COLS,), jnp.float32, 0.2, 0.8),
        'rw_w0_0': jax.random.uniform(next(ks), (RW_DIM,), jnp.float32, -6.0, -1.0),
        'rw_w2_0': nrm((DECAY_LORA, RW_DIM), DECAY_LORA ** -0.5),
        'rw_a0_0': nrm((RW_DIM,), 0.1),
        'rw_a2_0': nrm((AAA_LORA, RW_DIM), AAA_LORA ** -0.5),
        'rw_g2_0': nrm((GATE_LORA, RW_DIM), GATE_LORA ** -0.5),
        'rw_kk_0': 0.85 + nrm((RW_DIM,), 0.05),
        'rw_ka_0': 1.0 + nrm((RW_DIM,), 0.05),
        'rw_rk_0': nrm((RW_HEADS, HEAD_DIM), 0.1),
        'rw_lnx_g_0': gain(RW_DIM),
        'rw_lnx_b_0': nrm((RW_DIM,), 0.02),
        'mla_qnorm_0': gain(MLA_Q_RANK),
        'mla_wuq_0': nrm((MLA_Q_RANK, MLA_HEADS * (MLA_NOPE + MLA_ROPE)), MLA_Q_RANK ** -0.5),
        'mla_kvnorm_0': gain(MLA_KV_RANK),
        'mla_wukv_0': nrm((MLA_KV_RANK, MLA_HEADS * (MLA_NOPE + MLA_V)), MLA_KV_RANK ** -0.5),
        'w_out_0': nrm((MIX0_OUT, D), MIX0_OUT ** -0.5),
        'norm_ffn_0': gain(D),
        'ffn_wg_0': nrm((D, D_FF), D ** -0.5),
        'ffn_wu_0': nrm((D, D_FF), D ** -0.5),
        'ffn_wd_0': nrm((D_FF, D), D_FF ** -0.5),
        'norm_mix_1': gain(D),
        'w_in_1': nrm((D, IN1_COLS), D ** -0.5),
        'fox_bf_1': 3.0 + nrm((FOX_HEADS,), 0.5),
        'w_out_1': nrm((MIX1_OUT, D), MIX1_OUT ** -0.5),
        'norm_ffn_1': gain(D),
        'router_1': nrm((D, N_EXPERTS), D ** -0.5),
        'moe_wg_1': nrm((N_EXPERTS, D, D_EXPERT), D ** -0.5),
        'moe_wu_1': nrm((N_EXPERTS, D, D_EXPERT), D ** -0.5),
        'moe_wd_1': nrm((N_EXPERTS, D_EXPERT, D), D_EXPERT ** -0.5),
        'final_norm': gain(D),
    }


def reference(x, norm_mix_0, w_in_0, shift_mu_0, rw_w0_0, rw_w2_0, rw_a0_0, rw_a2_0, rw_g2_0,
              rw_kk_0, rw_ka_0, rw_rk_0, rw_lnx_g_0, rw_lnx_b_0, mla_qnorm_0, mla_wuq_0,
              mla_kvnorm_0, mla_wukv_0, w_out_0, norm_ffn_0, ffn_wg_0, ffn_wu_0, ffn_wd_0,
              norm_mix_1, w_in_1, fox_bf_1, w_out_1, norm_ffn_1, router_1, moe_wg_1, moe_wu_1,
              moe_wd_1, final_norm):
    seq_len = x.shape[1]
    rope_mla = rope_tables(seq_len, MLA_ROPE)
    rope_full = rope_tables(seq_len, HEAD_DIM)
    layer_params = (
        (norm_mix_0, w_in_0, shift_mu_0, rw_w0_0, rw_w2_0, rw_a0_0, rw_a2_0, rw_g2_0, rw_kk_0,
         rw_ka_0, rw_rk_0, rw_lnx_g_0, rw_lnx_b_0, mla_qnorm_0, mla_wuq_0, mla_kvnorm_0,
         mla_wukv_0, w_out_0, norm_ffn_0, ffn_wg_0, ffn_wu_0, ffn_wd_0),
        (norm_mix_1, w_in_1, fox_bf_1, w_out_1, norm_ffn_1, router_1, moe_wg_1, moe_wu_1,
         moe_wd_1),
    )
    h = x
    for layer in range(DEPTH):
        if layer % 2 == 0:
            h = even_layer(h, *layer_params[layer], *rope_mla)
        else:
            h = odd_layer(h, *layer_params[layer], *rope_full)
    return rmsnorm(h, final_norm)
```

```python
import numpy as np
import concourse.bass as bass
import concourse.mybir as mybir
from concourse.bass_utils import run_bass_kernel_spmd

F32 = mybir.dt.float32
BF16 = mybir.dt.bfloat16
AF = mybir.ActivationFunctionType
ALU = mybir.AluOpType
AX = mybir.AxisListType

ENGS = ('pe', 'act', 'dve', 'pool', 'sp')


class Trk:
    __slots__ = ('w', 'r', 'excl')

    def __init__(self, excl=False):
        self.w = None
        self.r = {}
        self.excl = excl


class Prog:
    def __init__(self, nc):
        self.nc = nc
        self.sem = {e: nc.alloc_semaphore("sem_" + e) for e in ENGS if e != 'sp'}
        self.cnt = {e: 0 for e in ENGS}
        self.waited = {e: {} for e in ENGS}
        self.ops = {e: [] for e in ENGS}
        self.dsem = {}
        self.dcnt = {}
        self.nbuf = 0
        self.same_engine_sync = True
        self.prefix = ""

    def sb(self, shape, dtype, name=None):
        self.nbuf += 1
        return self.nc.alloc_sbuf_tensor(self.prefix + (name or f"sb{self.nbuf}"), list(shape), dtype)

    def ps(self, shape, dtype=F32, name=None):
        self.nbuf += 1
        return self.nc.alloc_psum_tensor(self.prefix + (name or f"ps{self.nbuf}"), list(shape), dtype)

    def dma_sem(self, name):
        if name not in self.dsem:
            self.dsem[name] = self.nc.alloc_semaphore("dsem_" + name)
            self.dcnt[name] = 0
        return name

    def _waits(self, eng, reads, writes, force_own=False):
        need = {}
        for t in reads:
            if t.w is not None:
                s, v = t.w
                if need.get(s, 0) < v:
                    need[s] = v
            if t.excl:
                for s, v in t.r.items():
                    if need.get(s, 0) < v:
                        need[s] = v
        for t in writes:
            if t.w is not None:
                s, v = t.w
                if need.get(s, 0) < v:
                    need[s] = v
            for s, v in t.r.items():
                if need.get(s, 0) < v:
                    need[s] = v
        own = self.sem.get(eng)
        for s, v in need.items():
            if self.waited[eng].get(s, 0) >= v:
                continue
            if s is own and (eng == 'pe' or not self.same_engine_sync) and not force_own:
                continue
            self.ops[eng].append(('wait', s, v))
            self.waited[eng][s] = v

    def op(self, eng, fn, reads=(), writes=(), force_own=False):
        self._waits(eng, reads, writes, force_own)
        self.cnt[eng] += 1
        tag = (self.sem[eng], self.cnt[eng])
        self.ops[eng].append(('ins', fn, tag[0], 1))
        for t in reads:
            if t.r.get(tag[0], 0) < tag[1]:
                t.r[tag[0]] = tag[1]
        for t in writes:
            t.w = tag
            t.r = {}
        return tag

    def dma(self, q, semname, out, in_, reads=(), writes=(), **kw):
        self.dma_sem(semname)
        self._waits(q, reads, writes)
        self.dcnt[semname] += 16
        s = self.dsem[semname]
        tag = (s, self.dcnt[semname])
        self.ops[q].append(('ins', lambda e: e.dma_start(out=out, in_=in_, **kw), s, 16))
        for t in reads:
            if t.r.get(s, 0) < tag[1]:
                t.r[s] = tag[1]
        for t in writes:
            t.w = tag
            t.r = {}
        return tag

    def barrier(self):
        for e in ENGS:
            for e2, sm in self.sem.items():
                v = self.cnt[e2]
                if v > 0 and self.waited[e].get(sm, 0) < v:
                    self.ops[e].append(('wait', sm, v))
                    self.waited[e][sm] = v
            for name, sm in self.dsem.items():
                v = self.dcnt[name]
                if v > 0 and self.waited[e].get(sm, 0) < v:
                    self.ops[e].append(('wait', sm, v))
                    self.waited[e][sm] = v

    def wait_all(self, eng, trks):
        self._waits(eng, (), trks)

    def emit(self):
        nc = self.nc
        ops = self.ops

        def run(e, lst):
            for o in lst:
                if o[0] == 'wait':
                    e.wait_ge(o[1], o[2])
                else:
                    o[1](e).then_inc(o[2], o[3])

        with nc.Block() as block:
            @block.tensor
            def _(e):
                run(e, ops['pe'])

            @block.scalar
            def _(e):
                run(e, ops['act'])

            @block.vector
            def _(e):
                run(e, ops['dve'])

            @block.gpsimd
            def _(e):
                run(e, ops['pool'])

            @block.sync
            def _(e):
                run(e, ops['sp'])


def _mm(P, out, lhsT, rhs, start, stop, reads, writes, force_own=False):
    return P.op('pe', lambda e: e.matmul(out, lhsT=lhsT, rhs=rhs, start=start, stop=stop), reads, writes, force_own)


def _tr(P, out, in_, ident, reads, writes):
    return P.op('pe', lambda e: e.transpose(out, in_, ident), reads, writes)


def _act(P, out, in_, func, reads, writes, bias=None, scale=None, accum=None):
    kw = {}
    if bias is not None:
        kw['bias'] = bias
    if scale is not None:
        kw['scale'] = scale
    if accum is not None:
        kw['accum_out'] = accum
    return P.op('act', lambda e: e.activation(out=out, in_=in_, func=func, **kw), reads, writes)


def _tt(P, eng, out, in0, in1, op, reads, writes):
    return P.op(eng, lambda e: e.tensor_tensor(out=out, in0=in0, in1=in1, op=op), reads, writes)


def _ts(P, eng, out, in0, s1, s2, op0, op1, reads, writes, accum=None):
    if s2 is None:
        return P.op(eng, lambda e: e.tensor_scalar(out=out, in0=in0, scalar1=s1, scalar2=None, op0=op0), reads, writes)
    if accum is not None:
        return P.op(eng, lambda e: e.tensor_scalar(out=out, in0=in0, scalar1=s1, scalar2=s2, op0=op0, op1=op1, accum_out=accum), reads, writes)
    return P.op(eng, lambda e: e.tensor_scalar(out=out, in0=in0, scalar1=s1, scalar2=s2, op0=op0, op1=op1), reads, writes)


def _stt(P, eng, out, in0, scalar, in1, op0, op1, reads, writes):
    return P.op(eng, lambda e: e.scalar_tensor_tensor(out=out, in0=in0, scalar=scalar, in1=in1, op0=op0, op1=op1), reads, writes)


def _cp(P, eng, out, in_, reads, writes):
    if eng == 'act':
        return P.op('act', lambda e: e.copy(out=out, in_=in_), reads, writes)
    return P.op(eng, lambda e: e.tensor_copy(out=out, in_=in_), reads, writes)


def _memset(P, eng, ap, val, writes):
    return P.op(eng, lambda e: e.memset(ap, val), (), writes)


def _recip(P, out, in_, reads, writes):
    return P.op('dve', lambda e: e.reciprocal(out=out, in_=in_), reads, writes)


class Ring:
    def __init__(self, P, n, shape, dtype, psum=False, name=None):
        self.n = n
        self.bufs = []
        for j in range(n):
            t = (P.ps if psum else P.sb)(shape, dtype, name=(f"{name}_{j}" if name else None))
            self.bufs.append((t, Trk(excl=psum)))
        self.k = 0

    def next(self):
        b = self.bufs[self.k % self.n]
        self.k += 1
        return b


def make_consts(P):
    c = {}
    t = Trk()
    idb = P.sb([128, 128], BF16, "identb")
    idf = P.sb([128, 128], F32, "identf")
    tri = P.sb([128, 128], BF16, "tri")
    ones = P.sb([128, 128], BF16, "onesb")
    onesf = P.sb([128, 128], F32, "onesf")
    _memset(P, 'pool', idf[:], 1.0, [t])
    P.op('pool', lambda e: e.affine_select(out=idf[:], in_=idf[:], pattern=[[-1, 128]], compare_op=ALU.is_equal,
                                           fill=0.0, base=0, channel_multiplier=1), [t], [t])
    _cp(P, 'pool', idb[:], idf[:], [t], [t])
    _memset(P, 'pool', tri[:], 1.0, [t])
    P.op('pool', lambda e: e.affine_select(out=tri[:], in_=tri[:], pattern=[[1, 128]], compare_op=ALU.is_ge,
                                           fill=0.0, base=0, channel_multiplier=-1), [t], [t])
    _memset(P, 'pool', ones[:], 1.0, [t])
    _memset(P, 'pool', onesf[:], 1.0, [t])
    c.update(idb=idb, idf=idf, tri=tri, ones=ones, onesf=onesf, trk=t)
    return c


def load_w(P, W, K, N, gain=None, gtrk=None, name="w", stage=None, neg_cols=None):
    kc = K // 128
    wb = P.sb([128, kc, N], BF16, name)
    trk = Trk()
    if stage is None:
        stage = Ring(P, 2, [128, N], F32, name=name + "_st")
    for c in range(kc):
        st, strk = stage.next()
        P.dma('sp', f"{name}_st{(stage.k - 1) % stage.n}", st[:, 0:N], W[c * 128:(c + 1) * 128, :], writes=[strk])
        eng = 'act' if c % 2 == 0 else 'dve'
        if gain is not None:
            if eng == 'act':
                _act(P, wb[:, c, :], st[:, 0:N], AF.Copy, [strk, gtrk], [trk], scale=gain[:, c:c + 1])
            else:
                _ts(P, 'dve', wb[:, c, :], st[:, 0:N], gain[:, c:c + 1], None, ALU.mult, None, [strk, gtrk], [trk])
        else:
            _cp(P, eng, wb[:, c, :], st[:, 0:N], [strk], [trk])
        if neg_cols is not None:
            for (a, b) in neg_cols:
                _ts(P, 'dve', wb[:, c, a:b], wb[:, c, a:b], -1.0, None, ALU.mult, None, [trk], [trk])
    return wb, trk


class Front:
    def __init__(self, P, C, x, NT, eps=1e-6, D=1024, name="fe", n_uT=2, n_xs=3, n_pt=2):
        self.P, self.C, self.x, self.NT, self.eps, self.D = P, C, x, NT, eps, D
        self.kc = D // 128
        self.xs = Ring(P, n_xs, [128, D], F32, name=name + "_xs")
        self.n_xs = n_xs
        self.u = Ring(P, 2, [128, D], BF16, name=name + "_u")
        self.junk = P.sb([128, D], BF16, name + "_junk")
        self.junk_t = Trk()
        self.ss = Ring(P, 4, [128, 1], F32, name=name + "_ss")
        self.pt = Ring(P, n_pt, [128, D], BF16, psum=True, name=name + "_pt")
        self.uT = Ring(P, n_uT, [128, self.kc, NT], BF16, name=name + "_uT")
        self.name = name
        self.nev = 0

    def tile(self, i):
        P, C, NT, D = self.P, self.C, self.NT, self.D
        uT, uT_t = self.uT.next()
        for blk in range(NT // 128):
            r0 = i * NT + blk * 128
            xs, xs_t = self.xs.next()
            P.dma('sp', f"{self.name}_xs{(self.xs.k - 1) % self.n_xs}", xs[:], self.x[r0:r0 + 128, :], writes=[xs_t])
            ss, ss_t = self.ss.next()
            _act(P, self.junk[:], xs[:], AF.Square, [xs_t], [self.junk_t, ss_t], accum=ss[:])
            _ts(P, 'dve', ss[:], ss[:], 1.0 / D, self.eps, ALU.mult, ALU.add, [ss_t], [ss_t])
            _act(P, ss[:], ss[:], AF.Sqrt, [ss_t], [ss_t])
            _recip(P, ss[:], ss[:], [ss_t], [ss_t])
            u, u_t = self.u.next()
            _act(P, u[:], xs[:], AF.Copy, [xs_t, ss_t], [u_t], scale=ss[:, 0:1])
            pt, pt_t = self.pt.next()
            for c in range(self.kc):
                _tr(P, pt[:, c * 128:(c + 1) * 128], u[:, c * 128:(c + 1) * 128], C['idb'][:], [u_t, C['trk']], [pt_t])
            eng = 'dve' if self.nev % 2 == 0 else 'act'
            self.nev += 1
            _cp(P, eng, uT[:, :, blk * 128:(blk + 1) * 128], pt[:].rearrange("p (c t) -> p c t", t=128), [pt_t], [uT_t])
        return uT, uT_t


def proj_fm(P, ps, ps_t, W, W_t, c0, M, uT, uT_t, N, kc=8, n0=0):
    for c in range(kc):
        _mm(P, ps[0:M, 0:N], W[:, c, c0:c0 + M], uT[:, c, n0:n0 + N], c == 0, c == kc - 1, [W_t, uT_t], [ps_t])


def proj_tm(P, ps, ps_t, W, W_t, c0, ncols, uT, uT_t, t0, kc=8):
    for c in range(kc):
        _mm(P, ps[:, 0:ncols], uT[:, c, t0:t0 + 128], W[:, c, c0:c0 + ncols], c == 0, c == kc - 1, [W_t, uT_t], [ps_t])


class Attn:
    def __init__(self, P, C, NT, name="at", fox=False, n_pss=3, depth=1):
        self.P, self.C, self.NT = P, C, NT
        self.pss = Ring(P, n_pss, [128, NT], F32, psum=True, name=name + "_pss")
        self.depth = depth
        self.pT = Ring(P, 3, [128, NT], BF16, name=name + "_pT")
        self.po = Ring(P, 2, [128, NT], F32, psum=True, name=name + "_po")
        self.rden = Ring(P, 2, [128, NT], F32, name=name + "_rden")
        self.bcs = Ring(P, 2, [64, NT], F32, name=name + "_bcs")
        self.yo = Ring(P, 2, [64, NT], F32, name=name + "_yo")
        if fox:
            self.comb = Ring(P, 2, [65, NT], F32, name=name + "_comb")
            self.p2s = Ring(P, 1, [65, NT], F32, name=name + "_p2s")
        self.name = name
        self.nst = 0

    def head(self, i, KT, KT_trk, kp0, dk, QT, QT_t, Vtm, V_trk, vh, scale, y_dram, row0, fox=None, bg=None):
        P, C, NT = self.P, self.C, self.NT
        nq = NT // 128
        nkb = (i + 1) * nq
        ndiag0 = i * nq
        po, po_t = self.po.next()
        if fox is not None:
            po2, po2_t = self.po.next()

        def emit_S(kb):
            j = kb // nq
            off = max(0, kb - ndiag0) * 128
            ps, ps_t = self.pss.next()
            _mm(P, ps[:, off:NT], KT[kp0:kp0 + dk, kb * 128:(kb + 1) * 128], QT[kp0:kp0 + dk, off:NT], True, True,
                [KT_trk[j], QT_t], [ps_t])
            return ps, ps_t

        pend = [emit_S(kb) for kb in range(min(self.depth, nkb))]
        for kb in range(nkb):
            if kb + self.depth < nkb:
                pend.append(emit_S(kb + self.depth))
            ps, ps_t = pend.pop(0)
            if bg is not None:
                bg()
            j = kb // nq
            off = max(0, kb - ndiag0) * 128
            diag = kb >= ndiag0
            pT, pT_t = self.pT.next()
            if fox is None:
                _act(P, pT[:, off:NT], ps[:, off:NT], AF.Exp, [ps_t], [pT_t], scale=scale)
            elif not diag:
                _act(P, pT[:, off:NT], ps[:, off:NT], AF.Exp, [ps_t, fox['bt_t']], [pT_t], scale=scale, bias=fox['bt'][:, 0, kb:kb + 1])
            else:
                for qb in range(off // 128, nq):
                    _act(P, pT[:, qb * 128:(qb + 1) * 128], ps[:, qb * 128:(qb + 1) * 128], AF.Exp, [ps_t, fox['bt_t']], [pT_t],
                         scale=scale, bias=fox['bt'][:, qb, kb:kb + 1])
            if diag:
                eng = 'pool' if self.nst % 2 == 0 else 'dve'
                self.nst += 1
                _tt(P, eng, pT[:, off:off + 128], pT[:, off:off + 128], C['tri'][:], ALU.mult, [pT_t, C['trk']], [pT_t])
            if fox is not None and diag:
                _mm(P, po2[0:65, off:NT], Vtm[:, kb, vh, 0:65], pT[:, off:NT], kb == ndiag0, kb == nkb - 1, [V_trk[j], pT_t], [po2_t])
            elif fox is not None:
                _mm(P, po[0:65, off:NT], Vtm[:, kb, vh, 0:65], pT[:, off:NT], kb == 0, kb == ndiag0 - 1, [V_trk[j], pT_t], [po_t])
            else:
                _mm(P, po[0:65, off:NT], Vtm[:, kb, vh, 0:65], pT[:, off:NT], kb == 0, kb == nkb - 1, [V_trk[j], pT_t], [po_t])
        src, src_t = po, po_t
        if fox is not None:
            if i == 0:
                src, src_t = po2, po2_t
            else:
                p2s, p2s_t = self.p2s.next()
                _cp(P, 'act', p2s[:], po2[0:65, :], [po2_t], [p2s_t])
                cm, cm_t = self.comb.next()
                for qb in range(nq):
                    cs_ = slice(qb * 128, (qb + 1) * 128)
                    _stt(P, 'dve', cm[:, cs_], po[0:65, cs_], fox['f'][0:65, qb:qb + 1], p2s[:, cs_], ALU.mult, ALU.add,
                         [po_t, p2s_t, fox['f_t']], [cm_t])
                src, src_t = cm, cm_t
        rden, rden_t = self.rden.next()
        _recip(P, rden[64:65, :], src[64:65, :], [src_t], [rden_t])
        ps, ps_t = self.pss.next()
        P.op('pe', lambda e: e.matmul(ps[0:64, :], lhsT=C['onesf'][64:65, 0:64], rhs=rden[64:65, :], start=True, stop=True),
             [rden_t, C['trk']], [ps_t])
        bcs, bcs_t = self.bcs.next()
        _cp(P, 'act', bcs[:], ps[0:64, :], [ps_t], [bcs_t])
        yo, yo_t = self.yo.next()
        _tt(P, 'dve', yo[:], src[0:64, :], bcs[:], ALU.mult, [src_t, bcs_t], [yo_t])
        return yo, yo_t


class Env:
    def __init__(self, nc=None, P=None, C=None, mapping=None, prefix=""):
        self.fused = nc is not None
        self.nc = nc if nc is not None else bass.Bass("TRN2", target_bir_lowering=False)
        self.P = P if P is not None else Prog(self.nc)
        self.C = C
        self.mapping = mapping or {}
        self.prefix = prefix
        self.P.prefix = prefix

    def dram(self, name, shape, dtype, kind):
        if name in self.mapping:
            return self.mapping[name]
        return self.nc.dram_tensor(self.prefix + name, list(shape), dtype, kind=kind).ap()

    def consts(self):
        if self.C is None:
            self.C = make_consts(self.P)
        return self.C

    def finish(self, out_trks, eng):
        if self.fused:
            return self.nc
        self.P.wait_all(eng, out_trks)
        self.P.emit()
        return self.nc


D_MODEL = 1024
STOP = 0


def build_fox(S, NT=512, env=None):
    env = env or Env()
    nc = env.nc
    x = env.dram("x", [S, D_MODEL], F32, "ExternalInput")
    gain = env.dram("gain", [128, 8], F32, "ExternalInput")
    W = env.dram("W", [D_MODEL, 772], F32, "ExternalInput")
    bf = env.dram("bf", [4, 1], F32, "ExternalInput")
    yT = env.dram("yT", [256, S], F32, "ExternalOutput")
    P = env.P
    C = env.consts()
    nq = NT // 128
    ntile = S // NT
    nkb = S // 128
    g_sb = P.sb([128, 8], F32, "gain_sb"); g_t = Trk()
    P.dma('sp', 'misc', g_sb[:], gain, writes=[g_t])
    bf_sb = P.sb([4, 1], F32, "bf_sb"); bf_t = Trk()
    P.dma('sp', 'misc', bf_sb[:], bf, writes=[bf_t])
    Wb, W_t = load_w(P, W, D_MODEL, 772, g_sb, g_t, "Wb")
    fe = Front(P, C, x, NT, n_pt=1, n_xs=2)
    at = Attn(P, C, NT, fox=True)
    pp = Ring(P, 2, [128, NT], F32, psum=True, name="pp")
    KT = [P.sb([128, S], BF16, f"KT{hp}") for hp in range(2)]
    KT_trk = [[Trk() for _ in range(ntile)] for hp in range(2)]
    QT = Ring(P, 4, [128, NT], BF16, name="QT")
    Vtm = P.sb([128, nkb, 4, 65], BF16, "Vtm")
    V_trk = [Trk() for _ in range(ntile)]
    for j in range(ntile):
        _memset(P, 'pool', Vtm[:, j * nq:(j + 1) * nq, :, 64:65], 1.0, [V_trk[j]])
    Dt = Ring(P, 2, [4, NT], F32, name="Dt")
    lf = P.sb([4, NT], F32, "lf"); lf_t = Trk()
    onesrow = P.sb([4, NT], F32, "onesrow"); onesrow_t = Trk()
    _memset(P, 'pool', onesrow[:], 1.0, [onesrow_t])
    Dtm = P.sb([128, nkb, 4], F32, "Dtm")
    Dtm_trk = [Trk() for _ in range(ntile)]
    sel = P.sb([4, 4, 128], F32, "sel"); sel_t = Trk()
    for h in range(4):
        _cp(P, 'pool', sel[0:4, h, :], C['idf'][0:4, h:h + 1].to_broadcast([4, 128]), [C['trk']], [sel_t])
    cbc = Ring(P, 2, [128, 16], F32, name="cbc")
    biasT = Ring(P, 2, [128, nq, nkb], F32, name="biasT")
    fcol = Ring(P, 2, [128, nq], F32, name="fcol")
    out_t = Trk()
    prevD = None
    for i in range(ntile):
        uT, uT_t = fe.tile(i)
        qts = []
        for hp in range(2):
            ps, ps_t = pp.next()
            proj_fm(P, ps, ps_t, Wb, W_t, hp * 128, 128, uT, uT_t, NT)
            q, q_t = QT.next()
            _cp(P, 'act', q[:], ps[:], [ps_t], [q_t])
            qts.append((q, q_t))
            ps, ps_t = pp.next()
            proj_fm(P, ps, ps_t, Wb, W_t, 256 + hp * 128, 128, uT, uT_t, NT)
            _cp(P, 'dve', KT[hp][:, i * NT:(i + 1) * NT], ps[:], [ps_t], [KT_trk[hp][i]])
        for blk in range(nq):
            ps, ps_t = pp.next()
            proj_tm(P, ps, ps_t, Wb, W_t, 512, 256, uT, uT_t, blk * 128)
            _cp(P, 'act' if blk % 2 else 'dve', Vtm[:, i * nq + blk, :, 0:64], ps[:, 0:256].rearrange("p (h d) -> p h d", d=64),
                [ps_t], [V_trk[i]])
        ps, ps_t = pp.next()
        proj_fm(P, ps, ps_t, Wb, W_t, 768, 4, uT, uT_t, NT)
        _act(P, lf[:], ps[0:4, :], AF.Sigmoid, [ps_t, bf_t], [lf_t], bias=bf_sb[:, 0:1])
        _act(P, lf[:], lf[:], AF.Ln, [lf_t], [lf_t])
        D, D_t = Dt.next()
        if prevD is None:
            P.op('dve', lambda e, D=D: e.tensor_tensor_scan(out=D[:], data0=onesrow[:], data1=lf[:], initial=0.0,
                                                            op0=ALU.mult, op1=ALU.add), [onesrow_t, lf_t], [D_t])
        else:
            pD, pD_t = prevD
            P.op('dve', lambda e, D=D, pD=pD: e.tensor_tensor_scan(out=D[:], data0=onesrow[:], data1=lf[:],
                                                                   initial=pD[:, NT - 1:NT], op0=ALU.mult, op1=ALU.add),
                 [onesrow_t, lf_t, pD_t], [D_t])
        prevD = (D, D_t)
        for blk in range(nq):
            ps, ps_t = pp.next()
            _tr(P, ps[:, 0:4], D[0:4, blk * 128:(blk + 1) * 128], C['idf'][0:4, 0:4], [D_t, C['trk']], [ps_t])
            _cp(P, 'dve', Dtm[:, i * nq + blk, :], ps[:, 0:4], [ps_t], [Dtm_trk[i]])
        ps, ps_t = pp.next()
        for h in range(4):
            _mm(P, ps[:, h * nq:(h + 1) * nq], sel[0:4, h, :], D[0:4, 0:NT:128], True, True, [sel_t, D_t], [ps_t])
        cb, cb_t = cbc.next()
        _cp(P, 'act', cb[:, 0:4 * nq], ps[:, 0:4 * nq], [ps_t], [cb_t])
        nk = (i + 1) * nq
        for h in range(4):
            bt, bt_t = biasT.next()
            for qb in range(nq):
                _ts(P, 'pool', bt[:, qb, 0:nk], Dtm[:, 0:nk, h], -1.0, cb[:, h * nq + qb:h * nq + qb + 1], ALU.mult, ALU.add,
                    [cb_t] + Dtm_trk[0:i + 1], [bt_t])
            hp, hh = h // 2, h % 2
            q, q_t = qts[hp]
            fc, fc_t = fcol.next()
            _ts(P, 'dve', fc[:, 0:nq], cb[:, h * nq:(h + 1) * nq], cb[:, h * nq:h * nq + 1], None, ALU.subtract, None, [cb_t], [fc_t])
            _act(P, fc[:, 0:nq], fc[:, 0:nq], AF.Exp, [fc_t], [fc_t])
            yo, yo_t = at.head(i, KT[hp], KT_trk[hp], hh * 64, 64, q, q_t, Vtm, V_trk, h, 0.125, None, 0,
                               fox=dict(bt=bt, bt_t=bt_t, f=fc, f_t=fc_t))
            P.dma('pool', 'yst', yT[h * 64:(h + 1) * 64, i * NT:(i + 1) * NT], yo[:], reads=[yo_t], writes=[out_t])
    return env.finish([out_t], 'pool')


def build_mla(S, NT=512, env=None):
    env = env or Env()
    nc = env.nc
    x = env.dram("x", [S, D_MODEL], F32, "ExternalInput")
    gain = env.dram("gain", [128, 8], F32, "ExternalInput")
    W = env.dram("W", [D_MODEL, 576], F32, "ExternalInput")
    qg = env.dram("qg", [128, 2], F32, "ExternalInput")
    kvg = env.dram("kvg", [128, 1], F32, "ExternalInput")
    Wuq = env.dram("Wuq", [256, 768], F32, "ExternalInput")
    Wkv = env.dram("Wkv", [128, 512], F32, "ExternalInput")
    cs = env.dram("cs", [2, 32, S], F32, "ExternalInput")
    yT = env.dram("yT", [256, S], F32, "ExternalOutput")
    P = env.P
    C = env.consts()
    nq = NT // 128
    ntile = S // NT
    nkb = S // 128
    g_sb = P.sb([128, 8], F32, "gain_sb"); g_t = Trk()
    P.dma('sp', 'misc', g_sb[:], gain, writes=[g_t])
    qg_sb = P.sb([128, 2], F32, "qg_sb")
    P.dma('sp', 'misc', qg_sb[:], qg, writes=[g_t])
    kvg_sb = P.sb([128, 1], F32, "kvg_sb")
    P.dma('sp', 'misc', kvg_sb[:], kvg, writes=[g_t])
    stg = Ring(P, 2, [128, 768], F32, name="wstage")
    Wb, W_t = load_w(P, W, D_MODEL, 576, g_sb, g_t, "Wb", stage=stg, neg_cols=[(480 + 64, 480 + 64 + 16)])
    negq = [(384 + h * 96 + 64, 384 + h * 96 + 80) for h in range(4)]
    Wuqb, Wuq_t = load_w(P, Wuq, 256, 768, qg_sb, g_t, "Wuqb", stage=stg, neg_cols=negq)
    Wkvb, Wkv_t = load_w(P, Wkv, 128, 512, kvg_sb, g_t, "Wkvb", stage=stg)
    fe = Front(P, C, x, NT, n_xs=2, n_pt=1)
    at = Attn(P, C, NT, n_pss=4, depth=2)
    pp = Ring(P, 1, [128, NT], F32, psum=True, name="pp")
    KT = [P.sb([96, S], BF16, f"KT{h}") for h in range(4)]
    KT_trk = [[Trk() for _ in range(ntile)] for h in range(4)]
    QT = Ring(P, 8, [96, NT], BF16, name="QT")
    Vtm = P.sb([128, nkb, 4, 65], BF16, "Vtm")
    V_trk = [Trk() for _ in range(ntile)]
    for j in range(ntile):
        _memset(P, 'pool', Vtm[:, j * nq:(j + 1) * nq, :, 64:65], 1.0, [V_trk[j]])
    cst = Ring(P, 2, [96, 2, NT], F32, name="cst")
    qlb = Ring(P, 2, [128, 2, NT], BF16, name="qlb")
    kvb = Ring(P, 4, [128, NT], BF16, name="kvb")
    sq = Ring(P, 3, [128, NT], F32, name="sq")
    rq = Ring(P, 2, [128, NT], F32, name="rq")
    rkv = Ring(P, 2, [128, NT], F32, name="rkv")
    tmp = Ring(P, 3, [96, NT], F32, name="tmp")
    out_t = Trk()

    def rstd_bc(ps, ps_t, dst, dst_t, n):
        _ts(P, 'dve', dst[:], ps[:], 1.0 / n, 1e-6, ALU.mult, ALU.add, [ps_t], [dst_t])
        _act(P, dst[:], dst[:], AF.Sqrt, [dst_t], [dst_t])
        _recip(P, dst[:], dst[:], [dst_t], [dst_t])

    def prep(i):
        qs = []
        uT, uT_t = fe.tile(i)
        yield
        c_sb, c_t = cst.next()
        P.dma('sp', f"cs{(cst.k - 1) % 2}", c_sb[64:96, 0, :], cs[0, :, i * NT:(i + 1) * NT], writes=[c_t])
        P.dma('sp', f"cs{(cst.k - 1) % 2}", c_sb[64:96, 1, :], cs[1, :, i * NT:(i + 1) * NT], writes=[c_t])
        ql, ql_t = qlb.next()
        sqs = []
        for c in range(2):
            yield
            ps, ps_t = pp.next()
            proj_fm(P, ps, ps_t, Wb, W_t, c * 128, 128, uT, uT_t, NT)
            s_, s_t = sq.next()
            _act(P, s_[:], ps[:], AF.Square, [ps_t], [s_t])
            _cp(P, 'dve', ql[:, c, :], ps[:], [ps_t], [ql_t])
            sqs.append((s_, s_t))
        yield
        ps, ps_t = pp.next()
        for c in range(2):
            _mm(P, ps[:], C['onesf'][:], sqs[c][0][:], c == 0, c == 1, [C['trk'], sqs[c][1]], [ps_t])
        rq_sb, rq_t = rq.next()
        rstd_bc(ps, ps_t, rq_sb, rq_t, 256)
        yield
        ps, ps_t = pp.next()
        proj_fm(P, ps, ps_t, Wb, W_t, 256, 128, uT, uT_t, NT)
        s_, s_t = sq.next()
        _act(P, s_[:], ps[:], AF.Square, [ps_t], [s_t])
        kv, kv_t = kvb.next()
        _cp(P, 'dve', kv[:], ps[:], [ps_t], [kv_t])
        yield
        ps, ps_t = pp.next()
        _mm(P, ps[:], C['onesf'][:], s_[:], True, True, [C['trk'], s_t], [ps_t])
        rkv_sb, rkv_t = rkv.next()
        rstd_bc(ps, ps_t, rkv_sb, rkv_t, 128)
        kvn_, kvn_t = kvb.next()
        _tt(P, 'pool', kvn_[:], kv[:], rkv_sb[:], ALU.mult, [kv_t, rkv_t], [kvn_t])
        kv, kv_t = kvn_, kvn_t
        yield
        psa, psa_t = pp.next()
        proj_fm(P, psa, psa_t, Wb, W_t, 384, 96, uT, uT_t, NT)
        t1, t1_t = tmp.next()
        _tt(P, 'dve', t1[64:96, :], psa[64:96, :], c_sb[64:96, 0, :], ALU.mult, [psa_t, c_t], [t1_t])
        yield
        psb, psb_t = pp.next()
        proj_fm(P, psb, psb_t, Wb, W_t, 480, 96, uT, uT_t, NT)
        t2, t2_t = tmp.next()
        _tt(P, 'dve', t2[64:96, :], psb[64:96, :], c_sb[64:96, 1, :], ALU.mult, [psb_t, c_t], [t2_t])
        _tt(P, 'pool', t1[64:96, :], t1[64:96, :], t2[64:96, :], ALU.add, [t1_t, t2_t], [t1_t])
        for h in range(4):
            _cp(P, 'pool' if h % 2 else 'act', KT[h][64:96, i * NT:(i + 1) * NT], t1[64:96, :], [t1_t], [KT_trk[h][i]])
        for blk in range(nq):
            yield
            ps, ps_t = pp.next()
            _mm(P, ps[:, 0:256], kv[:, blk * 128:(blk + 1) * 128], Wkvb[:, 0, 256:512], True, True, [kv_t, Wkv_t], [ps_t])
            _cp(P, 'act' if blk % 2 else 'dve', Vtm[:, i * nq + blk, :, 0:64], ps[:, 0:256].rearrange("p (h d) -> p h d", d=64),
                [ps_t], [V_trk[i]])
        for h in range(4):
            yield
            ps, ps_t = pp.next()
            _mm(P, ps[0:64, :], Wkvb[:, 0, h * 64:(h + 1) * 64], kv[:], True, True, [kv_t, Wkv_t], [ps_t])
            _cp(P, 'act', KT[h][0:64, i * NT:(i + 1) * NT], ps[0:64, :], [ps_t], [KT_trk[h][i]])
            q, q_t = QT.next()
            yield
            psq, psq_t = pp.next()
            for c in range(2):
                _mm(P, psq[0:96, :], Wuqb[:, c, h * 96:(h + 1) * 96], ql[:, c, :], c == 0, c == 1, [Wuq_t, ql_t], [psq_t])
            _tt(P, 'dve', q[0:64, :], psq[0:64, :], rq_sb[0:64, :], ALU.mult, [psq_t, rq_t], [q_t])
            t1, t1_t = tmp.next()
            _tt(P, 'dve', t1[64:96, :], psq[64:96, :], c_sb[64:96, 0, :], ALU.mult, [psq_t, c_t], [t1_t])
            yield
            psr, psr_t = pp.next()
            for c in range(2):
                _mm(P, psr[0:96, :], Wuqb[:, c, 384 + h * 96:384 + (h + 1) * 96], ql[:, c, :], c == 0, c == 1, [Wuq_t, ql_t], [psr_t])
            t2, t2_t = tmp.next()
            _tt(P, 'dve', t2[64:96, :], psr[64:96, :], c_sb[64:96, 1, :], ALU.mult, [psr_t, c_t], [t2_t])
            _tt(P, 'pool', t1[64:96, :], t1[64:96, :], t2[64:96, :], ALU.add, [t1_t, t2_t], [t1_t])
            _tt(P, 'pool', q[64:96, :], t1[64:96, :], rq_sb[64:96, :], ALU.mult, [t1_t, rq_t], [q_t])
            qs.append((q, q_t))
            yield


        res[i] = qs

    res = {}

    def drain(g):
        for _ in g:
            pass

    drain(prep(0))
    for i in range(ntile):
        g = prep(i + 1) if i + 1 < ntile else None

        def bg(g=g):
            if g is not None:
                next(g, None)
        for h in range(4):
            q, q_t = res[i][h]
            yo, yo_t = at.head(i, KT[h], KT_trk[h], 0, 96, q, q_t, Vtm, V_trk, h, 96 ** -0.5, None, 0, bg=bg)
            P.dma('pool', 'yst', yT[h * 64:(h + 1) * 64, i * NT:(i + 1) * NT], yo[:], reads=[yo_t], writes=[out_t])
        if g is not None:
            drain(g)
        del res[i]
    return env.finish([out_t], 'pool')


def rope_tab(S, dim):
    inv = (10000.0 ** (-np.arange(0, dim, 2, dtype=np.float32) / np.float32(dim))).astype(np.float32)
    ang = np.arange(S, dtype=np.float32)[:, None] * inv[None, :]
    c = np.cos(ang).astype(np.float32).T
    s = np.sin(ang).astype(np.float32).T
    return np.ascontiguousarray(np.stack([np.concatenate([c, c], 0), np.concatenate([s, s], 0)], 0))


def mla_inputs(xb, p, norm_mix, w_in, qnorm, wuq, kvnorm, wukv, S):
    RW = 1792
    qlat = w_in[:, RW:RW + 256]
    kvlat = w_in[:, RW + 256:RW + 384]
    kpe = w_in[:, RW + 384:RW + 416]
    z64 = np.zeros((1024, 64), np.float32)
    perm = (np.arange(32) + 16) % 32
    W = np.concatenate([qlat, kvlat, z64, kpe, z64, kpe[:, perm]], axis=1)
    heads = range(4 * p, 4 * p + 4)
    uq = np.concatenate([wuq[:, h * 96:(h + 1) * 96] for h in heads], axis=1)
    z = np.zeros((256, 64), np.float32)
    uqr = np.concatenate([np.concatenate([z, wuq[:, h * 96 + 64:(h + 1) * 96][:, perm]], axis=1) for h in heads], axis=1)
    wk = np.concatenate([wukv[:, h * 128:h * 128 + 64] for h in heads], axis=1)
    wv = np.concatenate([wukv[:, h * 128 + 64:(h + 1) * 128] for h in heads], axis=1)
    return {"x": xb, "gain": np.ascontiguousarray(norm_mix.reshape(8, 128).T), "W": np.ascontiguousarray(W),
            "qg": np.ascontiguousarray(qnorm.reshape(2, 128).T), "kvg": np.ascontiguousarray(kvnorm.reshape(1, 128).T),
            "Wuq": np.ascontiguousarray(np.concatenate([uq, uqr], axis=1)),
            "Wkv": np.ascontiguousarray(np.concatenate([wk, wv], axis=1)), "cs": rope_tab(S, 32)}


def build_moba(S, NT=512, env=None):
    env = env or Env()
    nc = env.nc
    x = env.dram("x", [S, D_MODEL], F32, "ExternalInput")
    gain = env.dram("gain", [128, 8], F32, "ExternalInput")
    W = env.dram("W", [D_MODEL, 1280], F32, "ExternalInput")
    cs = env.dram("cs", [2, 64, S], F32, "ExternalInput")
    oh = env.dram("oh", [32, S], BF16, "ExternalInput")
    yT = env.dram("yT", [256, S], F32, "ExternalOutput")
    P = env.P
    C = env.consts()
    nq = NT // 128
    ntile = S // NT
    nkb = S // 128
    BIG = 30000.0
    g_sb = P.sb([128, 8], F32, "gain_sb"); g_t = Trk()
    P.dma('sp', 'misc', g_sb[:], gain, writes=[g_t])
    neg = [(768 + h * 64, 768 + h * 64 + 32) for h in range(4)] + [(1024 + h * 64, 1024 + h * 64 + 32) for h in range(4)]
    Wb, W_t = load_w(P, W, D_MODEL, 1280, g_sb, g_t, "Wb", neg_cols=neg)
    fe = Front(P, C, x, NT, n_pt=1)
    at = Attn(P, C, NT)
    pp = Ring(P, 2, [128, NT], F32, psum=True, name="pp")
    KT = [P.sb([96, S], BF16, f"KT{h}") for h in range(4)]
    KT_trk = [[Trk() for _ in range(ntile)] for h in range(4)]
    for h in range(4):
        for j in range(ntile):
            P.dma('sp', 'ohld', KT[h][64:96, j * NT:(j + 1) * NT], oh[:, j * NT:(j + 1) * NT], writes=[KT_trk[h][j]])
    QT = Ring(P, 2, [96, NT], BF16, name="QT")
    Vtm = P.sb([128, nkb, 4, 65], BF16, "Vtm")
    V_trk = [Trk() for _ in range(ntile)]
    for j in range(ntile):
        _memset(P, 'pool', Vtm[:, j * nq:(j + 1) * nq, :, 64:65], 1.0, [V_trk[j]])
    cst = Ring(P, 2, [64, 2, NT], F32, name="cst")
    tmp = Ring(P, 4, [64, NT], F32, name="tmp")
    kmT = P.sb([64, 4, max(S // 256, 8)], F32, "kmT"); km_t = [Trk() for h in range(4)]
    gate = Ring(P, 2, [128, 32], F32, name="gate")
    mx = Ring(P, 2, [128, 8], F32, name="mx")
    penpad = Ring(P, 2, [128, 96], F32, name="penpad")
    for (pb, pb_t) in penpad.bufs:
        _memset(P, 'pool', pb[:], 0.0, [pb_t])
    out_t = Trk()

    def roped(h, col0, rot0, uT, uT_t, c_sb, c_t):
        ps, ps_t = pp.next()
        proj_fm(P, ps, ps_t, Wb, W_t, col0 + h * 64, 64, uT, uT_t, NT)
        t1, t1_t = tmp.next()
        _tt(P, 'dve', t1[:], ps[0:64, :], c_sb[:, 0, :], ALU.mult, [ps_t, c_t], [t1_t])
        ps, ps_t = pp.next()
        proj_fm(P, ps, ps_t, Wb, W_t, rot0 + h * 64, 64, uT, uT_t, NT)
        t2, t2_t = tmp.next()
        _tt(P, 'dve', t2[:], ps[0:64, :], c_sb[:, 1, :], ALU.mult, [ps_t, c_t], [t2_t])
        _tt(P, 'pool', t1[:], t1[:], t2[:], ALU.add, [t1_t, t2_t], [t1_t])
        return t1, t1_t

    for i in range(ntile):
        uT, uT_t = fe.tile(i)
        c_sb, c_t = cst.next()
        P.dma('sp', f"cs{(cst.k - 1) % 2}", c_sb[:, 0, :], cs[0, :, i * NT:(i + 1) * NT], writes=[c_t])
        P.dma('sp', f"cs{(cst.k - 1) % 2}", c_sb[:, 1, :], cs[1, :, i * NT:(i + 1) * NT], writes=[c_t])
        for blk in range(nq):
            ps, ps_t = pp.next()
            proj_tm(P, ps, ps_t, Wb, W_t, 512, 256, uT, uT_t, blk * 128)
            _cp(P, 'act' if blk % 2 else 'dve', Vtm[:, i * nq + blk, :, 0:64], ps[:, 0:256].rearrange("p (h d) -> p h d", d=64),
                [ps_t], [V_trk[i]])
        for h in range(4):
            kf, kf_t = roped(h, 256, 1024, uT, uT_t, c_sb, c_t)
            _cp(P, 'act', KT[h][0:64, i * NT:(i + 1) * NT], kf[:], [kf_t], [KT_trk[h][i]])
            nb = NT // 256
            P.op('dve', lambda e, kf=kf, h=h, i=i, nb=nb: e.tensor_reduce(
                out=kmT[:, h, i * nb:(i + 1) * nb], in_=kf[:].rearrange("p (n t) -> p n t", t=256), axis=AX.X, op=ALU.add),
                [kf_t], [km_t[h]])
            _ts(P, 'dve', kmT[:, h, i * nb:(i + 1) * nb], kmT[:, h, i * nb:(i + 1) * nb], 1.0 / 256, None, ALU.mult, None,
                [km_t[h]], [km_t[h]])
            qf, qf_t = roped(h, 0, 768, uT, uT_t, c_sb, c_t)
            q, q_t = QT.next()
            _cp(P, 'act', q[0:64, :], qf[:], [qf_t], [q_t])
            for qb in range(nq):
                own = (i * NT + qb * 128) // 256
                pb, pb_t = penpad.next()
                if own >= 3:
                    ps, ps_t = pp.next()
                    _mm(P, ps[:, 0:own], qf[:, qb * 128:(qb + 1) * 128], kmT[:, h, 0:own], True, True, [qf_t, km_t[h]], [ps_t])
                    g_, g_t2 = gate.next()
                    _memset(P, 'pool', g_[:], -1e30, [g_t2])
                    _cp(P, 'dve', g_[:, 0:own], ps[:, 0:own], [ps_t], [g_t2])
                    m_, m_t = mx.next()
                    P.op('dve', lambda e, m_=m_, g_=g_: e.max(out=m_[:], in_=g_[:]), [g_t2], [m_t])
                    _ts(P, 'dve', g_[:], g_[:], m_[:, 2:3], None, ALU.is_ge, None, [g_t2, m_t], [g_t2])
                    _ts(P, 'dve', pb[:, 64:96], g_[:], -1.0, BIG, ALU.add, ALU.mult, [g_t2], [pb_t])
                    if own < 32:
                        _memset(P, 'dve', pb[:, 64 + own:96], 0.0, [pb_t])
                else:
                    _memset(P, 'dve', pb[:, 64:96], 0.0, [pb_t])
                ps, ps_t = pp.next()
                _tr(P, ps[0:96, 0:128], pb[:, 0:96], C['idf'][:], [pb_t, C['trk']], [ps_t])
                _cp(P, 'act', q[64:96, qb * 128:(qb + 1) * 128], ps[64:96, 0:128], [ps_t], [q_t])
            yo, yo_t = at.head(i, KT[h], KT_trk[h], 0, 96, q, q_t, Vtm, V_trk, h, 0.125, None, 0)
            P.dma('pool', 'yst', yT[h * 64:(h + 1) * 64, i * NT:(i + 1) * NT], yo[:], reads=[yo_t], writes=[out_t])
    return env.finish([out_t], 'pool')


def moba_inputs(xb, p, norm_mix, w_in, S):
    import ml_dtypes
    base = 1544
    hs = slice(p * 256, (p + 1) * 256)
    q = w_in[:, base:base + 512][:, hs]
    k = w_in[:, base + 512:base + 1024][:, hs]
    v = w_in[:, base + 1024:base + 1536][:, hs]
    perm = np.concatenate([h * 64 + (np.arange(64) + 32) % 64 for h in range(4)])
    W = np.concatenate([q, k, v, q[:, perm], k[:, perm]], axis=1)
    oh = np.zeros((32, S), np.float32)
    for n in range(min(32, S // 256)):
        oh[n, n * 256:(n + 1) * 256] = 1.0
    return {"x": xb, "gain": np.ascontiguousarray(norm_mix.reshape(8, 128).T), "W": np.ascontiguousarray(W),
            "cs": rope_tab(S, 64), "oh": oh.astype(ml_dtypes.bfloat16)}


def build_tok(T, moe, NE, DFF, TB=2048, FG=256, env=None, sel=False):
    env = env or Env()
    nc = env.nc
    TB = min(TB, T)
    TX = T * (2 if sel else 1)
    x = env.dram("x", [TX, D_MODEL], F32, "ExternalInput")
    yT = env.dram("yT", [D_MODEL, TX], F32, "ExternalInput")
    if sel:
        psel = env.dram("psel", [128, 2], F32, "ExternalInput")
    Wout = env.dram("Wout", [D_MODEL, D_MODEL], F32, "ExternalInput")
    gff = env.dram("gff", [1, D_MODEL], F32, "ExternalInput")
    Wg = env.dram("Wg", [NE, D_MODEL, DFF], F32, "ExternalInput")
    Wu = env.dram("Wu", [NE, D_MODEL, DFF], F32, "ExternalInput")
    Wd = env.dram("Wd", [NE, DFF, D_MODEL], F32, "ExternalInput")
    if moe:
        router = env.dram("router", [D_MODEL, 8], F32, "ExternalInput")
        fg_d = env.dram("fgain", [1, D_MODEL], F32, "ExternalInput")
    out = env.dram("out", [T, D_MODEL], F32, "ExternalOutput")
    P = env.P
    C = env.consts()
    D = D_MODEL
    nb = TB // 128
    nsub = TB // 512
    npass = T // TB
    ngrp = DFF // FG
    nj = FG // 128
    m_t = Trk()
    gbc = P.sb([128, D], F32, "gbc")
    P.dma('sp', 'misc', gbc[:], gff.partition_broadcast(128), writes=[m_t])
    if moe:
        fgbc = P.sb([128, D], F32, "fgbc")
        P.dma('sp', 'misc', fgbc[:], fg_d.partition_broadcast(128), writes=[m_t])
        rt_sb = P.sb([128, 8, 8], F32, "rt_sb")
        P.dma('sp', 'misc', rt_sb[:], router.rearrange("(c p) e -> p c e", p=128), writes=[m_t])
    if sel:
        psel_sb = P.sb([128, 2], F32, "psel_sb")
        P.dma('sp', 'misc', psel_sb[:], psel, writes=[m_t])
        xB = Ring(P, 1, [128, D], F32, name="xB")
        yA = Ring(P, 2, [128, 512], BF16, name="yA")
        yB = Ring(P, 2, [128, 512], BF16, name="yB")
    Woutb = P.sb([128, 8, D], BF16, "Woutb"); Wout_t = Trk()
    for c in range(8):
        P.dma('pool', 'wout', Woutb[:, c, :], Wout[c * 128:(c + 1) * 128, :], writes=[Wout_t])
    H = P.sb([128, nb, D], F32, "H")
    H_t = [Trk() for _ in range(nb)]
    u2T = P.sb([128, 8, TB], BF16, "u2T")
    u2T_t = [Trk() for _ in range(nb)]
    yTb = Ring(P, 1, [128, 8, 512], BF16, name="yTb")
    udt = F32 if moe else BF16
    u = Ring(P, 1 if moe else 2, [128, D], udt, name="u")
    junk = P.sb([128, D], BF16, "junk"); junk_t = Trk()
    ss = Ring(P, 4, [128, 1], F32, name="ss")
    pt = Ring(P, 1 if moe else 2, [128, D], udt, psum=True, name="pt")
    psA = Ring(P, 2, [128, 512], F32, psum=True, name="psA")
    psg = Ring(P, 2, [128, 512], F32, psum=True, name="psg")
    psu = Ring(P, 2, [128, 512], F32, psum=True, name="psu")
    wg = Ring(P, 3, [128, 8, FG], BF16, name="wg")
    wu = Ring(P, 3, [128, 8, FG], BF16, name="wu")
    wd = Ring(P, 3, [128, nj, D], BF16, name="wd")
    actT = Ring(P, 2, [128, nj, TB], BF16, name="actT")
    stmp = Ring(P, 2, [128, 512], F32, name="stmp")
    if moe:
        uTf = Ring(P, 1, [128, 8, 128], F32, name="uTf")
        comb = P.sb([128, nb, 8], F32, "comb"); comb_t = [Trk() for _ in range(nb)]
        lg = Ring(P, 2, [128, 8], F32, name="lg")
        mx = Ring(P, 2, [128, 8], F32, name="mx")
        gt = Ring(P, 2, [128, 4], F32, name="gt")
    ident = C['idf'] if moe else C['idb']
    out_t = Trk()
    nev = 0
    for ps_ in range(npass):
        t0 = ps_ * TB
        for sub in range(nsub):
            yb, yb_t = yTb.next()
            if sel:
                for c in range(8):
                    ya, ya_t = yA.next(); yb2, yb2_t = yB.next()
                    sl = (yA.k - 1) % 2
                    cA = t0 + sub * 512
                    P.dma('pool', f"yA{sl}", ya[:], yT[c * 128:(c + 1) * 128, cA:cA + 512], writes=[ya_t])
                    P.dma('pool', f"yB{sl}", yb2[:], yT[c * 128:(c + 1) * 128, T + cA:T + cA + 512], writes=[yb2_t])
                    _ts(P, 'dve', ya[:], ya[:], psel_sb[:, 0:1], None, ALU.mult, None, [ya_t, m_t], [ya_t])
                    _stt(P, 'dve', yb[:, c, :], yb2[:], psel_sb[:, 1:2], ya[:], ALU.mult, ALU.add, [yb2_t, ya_t, m_t], [yb_t])
            else:
                P.dma('pool', 'ytb', yb[:], yT[:, t0 + sub * 512:t0 + (sub + 1) * 512].rearrange("(c p) t -> p c t", p=128), writes=[yb_t])
            for blk in range(4):
                b = sub * 4 + blk
                r0 = t0 + b * 128
                P.dma('sp', f"xld{b % 4}", H[:, b, :], x[r0:r0 + 128, :], writes=[H_t[b]])
                if sel:
                    xb_, xb_t = xB.next()
                    P.dma('sp', "xldB", xb_[:], x[T + r0:T + r0 + 128, :], writes=[xb_t])
                    _ts(P, 'dve', H[:, b, :], H[:, b, :], psel_sb[:, 0:1], None, ALU.mult, None, [H_t[b], m_t], [H_t[b]])
                    _stt(P, 'dve', H[:, b, :], xb_[:], psel_sb[:, 1:2], H[:, b, :], ALU.mult, ALU.add, [xb_t, H_t[b], m_t], [H_t[b]])
                for half in range(2):
                    ps, ps_t = psA.next()
                    for c in range(8):
                        _mm(P, ps[:], yb[:, c, blk * 128:(blk + 1) * 128], Woutb[:, c, half * 512:(half + 1) * 512], c == 0, c == 7,
                            [yb_t, Wout_t], [ps_t])
                    _tt(P, 'dve', H[:, b, half * 512:(half + 1) * 512], ps[:], H[:, b, half * 512:(half + 1) * 512], ALU.add,
                        [ps_t, H_t[b]], [H_t[b]])
                s_, s_t = ss.next()
                _act(P, junk[:], H[:, b, :], AF.Square, [H_t[b]], [junk_t, s_t], accum=s_[:])
                _ts(P, 'dve', s_[:], s_[:], 1.0 / D, 1e-6, ALU.mult, ALU.add, [s_t], [s_t])
                _act(P, s_[:], s_[:], AF.Sqrt, [s_t], [s_t])
                _recip(P, s_[:], s_[:], [s_t], [s_t])
                u_, u_t = u.next()
                _stt(P, 'dve', u_[:], H[:, b, :], s_[:, 0:1], gbc[:], ALU.mult, ALU.mult, [H_t[b], s_t, m_t], [u_t])
                p_, p_t = pt.next()
                for c in range(8):
                    _tr(P, p_[:, c * 128:(c + 1) * 128], u_[:, c * 128:(c + 1) * 128], ident[:], [u_t, C['trk']], [p_t])
                if moe:
                    uf, uf_t = uTf.next()
                    _cp(P, 'act', uf[:], p_[:].rearrange("p (c t) -> p c t", t=128), [p_t], [uf_t])
                    _cp(P, 'dve', u2T[:, :, b * 128:(b + 1) * 128], uf[:], [uf_t], [u2T_t[b]])
                    ps, ps_t = psA.next()
                    for c in range(8):
                        _mm(P, ps[:, 0:8], uf[:, c, :], rt_sb[:, c, :], c == 0, c == 7, [uf_t, m_t], [ps_t])
                    l_, l_t = lg.next()
                    _cp(P, 'act', l_[:], ps[:, 0:8], [ps_t], [l_t])
                    m_, mm_t = mx.next()
                    P.op('dve', lambda e, m_=m_, l_=l_: e.max(out=m_[:], in_=l_[:]), [l_t], [mm_t])
                    g_, g_t = gt.next()
                    _tt(P, 'dve', g_[:, 0:1], m_[:, 0:1], m_[:, 1:2], ALU.subtract, [mm_t], [g_t])
                    _act(P, g_[:, 1:2], g_[:, 0:1], AF.Sigmoid, [g_t], [g_t], scale=-1.0)
                    _act(P, g_[:, 0:1], g_[:, 0:1], AF.Sigmoid, [g_t], [g_t])
                    _ts(P, 'dve', comb[:, b, :], l_[:], m_[:, 0:1], g_[:, 0:1], ALU.is_equal, ALU.mult, [l_t, mm_t, g_t], [comb_t[b]])
                    _ts(P, 'dve', l_[:], l_[:], m_[:, 1:2], g_[:, 1:2], ALU.is_equal, ALU.mult, [l_t, mm_t, g_t], [l_t])
                    _tt(P, 'dve', comb[:, b, :], comb[:, b, :], l_[:], ALU.add, [l_t, comb_t[b]], [comb_t[b]])
                else:
                    eng = 'dve' if nev % 2 == 0 else 'act'
                    nev += 1
                    _cp(P, eng, u2T[:, :, b * 128:(b + 1) * 128], p_[:].rearrange("p (c t) -> p c t", t=128), [p_t], [u2T_t[b]])
        for e in range(NE):
            for fg in range(ngrp):
                f0 = fg * FG
                wg_, wg_t = wg.next(); wu_, wu_t = wu.next(); wd_, wd_t = wd.next()
                sl = (wg.k - 1) % 3
                P.dma('pool', f"wg{sl}", wg_[:], Wg[e, :, f0:f0 + FG].rearrange("(c p) f -> p c f", p=128), writes=[wg_t])
                P.dma('pool', f"wu{sl}", wu_[:], Wu[e, :, f0:f0 + FG].rearrange("(c p) f -> p c f", p=128), writes=[wu_t])
                P.dma('pool', f"wd{sl}", wd_[:], Wd[e, f0:f0 + FG, :].rearrange("(j p) d -> p j d", p=128), writes=[wd_t])
                a_, a_t = actT.next()
                for j in range(nj):
                    for n in range(TB // 512):
                        ut = u2T_t[n * 4:(n + 1) * 4]
                        pg, pg_t = psg.next()
                        for c in range(8):
                            _mm(P, pg[:], wg_[:, c, j * 128:(j + 1) * 128], u2T[:, c, n * 512:(n + 1) * 512], c == 0, c == 7, [wg_t] + ut, [pg_t])
                        pu, pu_t = psu.next()
                        for c in range(8):
                            _mm(P, pu[:], wu_[:, c, j * 128:(j + 1) * 128], u2T[:, c, n * 512:(n + 1) * 512], c == 0, c == 7, [wu_t] + ut, [pu_t])
                        st_, st_t = stmp.next()
                        _act(P, st_[:], pg[:], AF.Silu, [pg_t], [st_t])
                        _tt(P, 'dve', a_[:, j, n * 512:(n + 1) * 512], st_[:], pu[:], ALU.mult, [st_t, pu_t], [a_t])
                for b in range(nb):
                    for half in range(2):
                        ps, ps_t = psA.next()
                        for j in range(nj):
                            _mm(P, ps[:], a_[:, j, b * 128:(b + 1) * 128], wd_[:, j, half * 512:(half + 1) * 512], j == 0, j == nj - 1,
                                [a_t, wd_t], [ps_t])
                        hs = H[:, b, half * 512:(half + 1) * 512]
                        if moe:
                            _stt(P, 'dve', hs, ps[:], comb[:, b, e:e + 1], hs, ALU.mult, ALU.add, [ps_t, comb_t[b], H_t[b]], [H_t[b]])
                        else:
                            _tt(P, 'dve', hs, ps[:], hs, ALU.add, [ps_t, H_t[b]], [H_t[b]])
        for b in range(nb):
            r0 = t0 + b * 128
            if moe:
                s_, s_t = ss.next()
                _act(P, junk[:], H[:, b, :], AF.Square, [H_t[b]], [junk_t, s_t], accum=s_[:])
                _ts(P, 'dve', s_[:], s_[:], 1.0 / D, 1e-6, ALU.mult, ALU.add, [s_t], [s_t])
                _act(P, s_[:], s_[:], AF.Sqrt, [s_t], [s_t])
                _recip(P, s_[:], s_[:], [s_t], [s_t])
                _stt(P, 'dve', H[:, b, :], H[:, b, :], s_[:, 0:1], fgbc[:], ALU.mult, ALU.mult, [H_t[b], s_t, m_t], [H_t[b]])
            P.dma('sp', 'ost', out[r0:r0 + 128, :], H[:, b, :], reads=[H_t[b]], writes=[out_t])
    return env.finish([out_t], 'sp')


def build_rwkv(S, NT=512, env=None):
    env = env or Env()
    nc = env.nc
    x = env.dram("x", [S, D_MODEL], F32, "ExternalInput")
    gain = env.dram("gain", [128, 8], F32, "ExternalInput")
    W = env.dram("W", [D_MODEL, 1024], F32, "ExternalInput")
    mu = env.dram("mu", [128, 8], F32, "ExternalInput")
    prm = env.dram("prm", [128, 2, 8], F32, "ExternalInput")
    w2 = env.dram("w2", [64, 256], F32, "ExternalInput")
    a2 = env.dram("a2", [64, 256], F32, "ExternalInput")
    g2 = env.dram("g2", [128, 256], F32, "ExternalInput")
    yT = env.dram("yT", [256, S], F32, "ExternalOutput")
    P = env.P
    C = env.consts()
    idf = C['idf']
    NC = NT // 64
    ntile = S // NT
    m_t = Trk()
    g_sb = P.sb([128, 8], F32, "gain_sb")
    P.dma('sp', 'misc', g_sb[:], gain, writes=[m_t])
    mu_sb = P.sb([128, 8], F32, "mu_sb")
    P.dma('sp', 'misc', mu_sb[:], mu, writes=[m_t])
    prm_sb = P.sb([128, 2, 8], F32, "prm_sb")
    P.dma('sp', 'misc', prm_sb[:], prm, writes=[m_t])
    w2_sb = P.sb([64, 256], F32, "w2_sb")
    P.dma('sp', 'misc', w2_sb[:], w2, writes=[m_t])
    a2_sb = P.sb([128, 256], F32, "a2_sb")
    P.dma('sp', 'misc', a2_sb[64:128, :], a2, writes=[m_t])
    g2_sb = P.sb([128, 256], F32, "g2_sb")
    P.dma('sp', 'misc', g2_sb[:], g2, writes=[m_t])
    _ts(P, 'dve', prm_sb[:, :, 7:8], prm_sb[:, :, 3:4], -1.0, 1.0, ALU.mult, ALU.add, [m_t], [m_t])
    Wb, W_t = load_w(P, W, D_MODEL, 1024, g_sb, m_t, "Wb")
    fe = Front(P, C, x, NT, n_uT=1, n_xs=2)
    PS = Ring(P, 5, [128, 512], F32, psum=True, name="PS")
    PSB = Ring(P, 1, [128, 1024], BF16, psum=True, name="PSB")
    k_t = Trk()
    bones = P.sb([128, 128], F32, "bones")
    _memset(P, 'pool', bones[:], 0.0, [k_t])
    _memset(P, 'pool', bones[0:64, 0:64], 1.0, [k_t])
    _memset(P, 'pool', bones[64:128, 64:128], 1.0, [k_t])
    smask = P.sb([128, NT], F32, "smask")
    _memset(P, 'pool', smask[:], 1.0, [k_t])
    _memset(P, 'pool', smask[:, 0:NT:64], 0.0, [k_t])
    mus = P.sb([64, 64], F32, "mus"); mui = P.sb([64, 64], F32, "mui"); mls = P.sb([64, 64], F32, "mls")
    for (t_, base, cm, pat) in ((mus, -1, -1, 1), (mui, 0, -1, 1), (mls, -1, 1, -1)):
        _memset(P, 'pool', t_[:], 1.0, [k_t])
        P.op('pool', lambda e, t_=t_, base=base, cm=cm, pat=pat: e.affine_select(
            out=t_[:], in_=t_[:], pattern=[[pat, 64]], compare_op=ALU.is_ge, fill=0.0, base=base, channel_multiplier=cm), [k_t], [k_t])
    maskA = P.sb([64, 2, 256], F32, "maskA")
    for j in range(2):
        for q_, src in enumerate((mus, mui, mus, mui)):
            _cp(P, 'pool', maskA[:, j, q_ * 64:(q_ + 1) * 64], src[:], [k_t], [k_t])
    maskP = P.sb([64, 8, 64], F32, "maskP")
    for j in range(8):
        _cp(P, 'pool', maskP[:, j, :], mls[:], [k_t], [k_t])
    id8 = P.sb([64, 8, 64], F32, "id8")
    for j in range(8):
        _cp(P, 'pool', id8[:, j, :], idf[0:64, 0:64], [C['trk']], [k_t])
    sh = [P.sb([128, NT + 1], F32, f"sh{c}") for c in range(8)]
    sh_t = [Trk() for _ in range(8)]
    for c in range(8):
        _memset(P, 'pool', sh[c][:, 0:1], 0.0, [sh_t[c]])
    X = [P.sb([128, NT], F32, f"X{c}") for c in range(8)]
    X_t = [Trk() for _ in range(8)]
    Hs = [P.sb([128, 64], F32, f"Hs{hp}") for hp in range(2)]
    Hs_t = [Trk() for _ in range(2)]
    for hp in range(2):
        _memset(P, 'pool', Hs[hp][:], 0.0, [Hs_t[hp]])
    names = ["th", "sgd", "lw", "a", "g", "kk", "kp", "bb", "L", "eL", "eLm", "eLp", "eC", "t1", "t2", "BT", "KTt", "KH", "BH",
             "bonus", "YT"]
    LP = BF16
    E = {n: P.sb([128, NT], LP if n in ("BT", "KTt", "KH", "BH") else F32, "e_" + n) for n in names}
    vb = P.sb([128, NT], LP, "vb"); vb_t = Trk()
    Hb = [P.sb([128, 64], LP, f"Hb{hp}") for hp in range(2)]
    Hb_t = [Trk() for _ in range(2)]
    for hp in range(2):
        _memset(P, 'pool', Hb[hp][:], 0.0, [Hb_t[hp]])
    Et = {n: Trk() for n in names}
    AR = P.sb([128, NC, 2, 64], LP, "AR"); AR_t = Trk()
    gC = P.sb([128, NC], F32, "gC"); gC_t = Trk()
    TM = {n: P.sb([64, NC, 128], LP, "tm_" + n) for n in ("V", "A", "KH", "BH")}
    TM_t = {n: Trk() for n in TM}
    M12 = [P.sb([64, NC, 256], LP, f"M12_{h}") for h in range(2)]; M12_t = [Trk(), Trk()]
    PQ = [[P.sb([64, NC, 128], LP, f"PQ{h}_{j}") for j in range(2)] for h in range(2)]; PQ_t = [[Trk(), Trk()], [Trk(), Trk()]]
    TT = [P.sb([64, NC, 64], LP, f"TT{h}") for h in range(2)]; TT_t = [Trk(), Trk()]
    Zs = [P.sb([64, NC, 64], LP, f"Zs{h}") for h in range(2)]; Zs_t = [Trk(), Trk()]
    W1T = P.sb([128, NC, 64], LP, "W1T"); W1T_t = Trk()
    U0s = P.sb([64, NC, 2, 64], F32, "U0s"); U0s_t = Trk()
    M12b = P.sb([64, 2, NC, 128], LP, "M12b"); M12b_t = Trk()
    Us = Ring(P, 2, [64, 128], LP, name="Us")
    Ysb = P.sb([64, NC, 128], F32, "Ysb"); Ysb_t = Trk()
    Ysq = P.sb([64, NC, 128], F32, "Ysq"); Ysq_t = Trk()
    st = P.sb([64, 4, NC * 2], F32, "gnst"); st_t = Trk()
    out_t = Trk()
    CE = -float(np.exp(-0.5))

    for i in range(ntile):
        uT, uT_t = fe.tile(i)
        for c in range(8):
            ps, ps_t = PS.next()
            proj_fm(P, ps, ps_t, Wb, W_t, c * 128, 128, uT, uT_t, NT)
            _cp(P, 'act', sh[c][:, 1:NT + 1], ps[:, 0:NT], [ps_t], [sh_t[c]])
            _tt(P, 'dve', E["t1"][:], sh[c][:, 0:NT], sh[c][:, 1:NT + 1], ALU.subtract, [sh_t[c]], [Et["t1"]])
            _stt(P, 'dve', X[c][:], E["t1"][:], mu_sb[:, c:c + 1], sh[c][:, 1:NT + 1], ALU.mult, ALU.add, [Et["t1"], sh_t[c], m_t], [X_t[c]])
            _cp(P, 'act', sh[c][:, 0:1], sh[c][:, NT:NT + 1], [sh_t[c]], [sh_t[c]])
        if STOP == 21: break
        _act(P, E["th"][0:64, :], X[6][0:64, :], AF.Tanh, [X_t[6]], [Et["th"]])
        _act(P, E["sgd"][:], X[7][:], AF.Sigmoid, [X_t[7]], [Et["sgd"]])
        for hp in range(2):
            r, k, v = X[hp], X[2 + hp], X[4 + hp]
            r_t, k_tk, v_t = X_t[hp], X_t[2 + hp], X_t[4 + hp]
            pr = lambda j: prm_sb[:, hp, j:j + 1]
            hc = slice(hp * 128, (hp + 1) * 128)
            ps, ps_t = PS.next()
            _mm(P, ps[:, 0:NT], w2_sb[0:64, hc], E["th"][0:64, :], True, True, [m_t, Et["th"]], [ps_t])
            _act(P, E["lw"][:], ps[:, 0:NT], AF.Sigmoid, [ps_t, m_t], [Et["lw"]], bias=pr(0))
            _ts(P, 'dve', E["lw"][:], E["lw"][:], CE, None, ALU.mult, None, [Et["lw"]], [Et["lw"]])
            P.op('dve', lambda e: e.tensor_tensor_scan(out=E["L"][:], data0=smask[:], data1=E["lw"][:], initial=0.0,
                                                       op0=ALU.mult, op1=ALU.add), [k_t, Et["lw"]], [Et["L"]])
            ps, ps_t = PS.next()
            _mm(P, ps[:, 0:NT], a2_sb[64:128, hc], X[6][64:128, :], True, True, [m_t, X_t[6]], [ps_t])
            _act(P, E["a"][:], ps[:, 0:NT], AF.Sigmoid, [ps_t, m_t], [Et["a"]], bias=pr(1))
            ps, ps_t = PS.next()
            _mm(P, ps[:, 0:NT], g2_sb[:, hc], E["sgd"][:], True, True, [m_t, Et["sgd"]], [ps_t])
            _cp(P, 'act', E["g"][:], ps[:, 0:NT], [ps_t], [Et["g"]])
            _ts(P, 'dve', E["kk"][:], k[:], pr(2), None, ALU.mult, None, [k_tk, m_t], [Et["kk"]])
            _act(P, E["t1"][:], E["kk"][:], AF.Square, [Et["kk"]], [Et["t1"]])
            ps, ps_t = PS.next()
            _mm(P, ps[:, 0:NT], bones[:], E["t1"][:], True, True, [k_t, Et["t1"]], [ps_t])
            _act(P, E["t2"][:], ps[:, 0:NT], AF.Sqrt, [ps_t], [Et["t2"]])
            _ts(P, 'dve', E["t2"][:], E["t2"][:], 1e-12, None, ALU.max, None, [Et["t2"]], [Et["t2"]])
            _recip(P, E["t2"][:], E["t2"][:], [Et["t2"]], [Et["t2"]])
            _tt(P, 'pool', E["kk"][:], E["kk"][:], E["t2"][:], ALU.mult, [Et["kk"], Et["t2"]], [Et["kk"]])
            _ts(P, 'dve', E["t1"][:], E["a"][:], pr(3), pr(7), ALU.mult, ALU.add, [Et["a"], m_t], [Et["t1"]])
            _tt(P, 'pool', E["kp"][:], k[:], E["t1"][:], ALU.mult, [k_tk, Et["t1"]], [Et["kp"]])
            _tt(P, 'pool', E["bb"][:], E["kk"][:], E["a"][:], ALU.mult, [Et["kk"], Et["a"]], [Et["bb"]])
            L3 = E["L"][:].rearrange("p (c t) -> p c t", t=64)
            _act(P, E["eL"][:], E["L"][:], AF.Exp, [Et["L"]], [Et["eL"]])
            _act(P, E["eLm"][:], E["L"][:], AF.Exp, [Et["L"]], [Et["eLm"]], scale=-1.0)
            _tt(P, 'dve', E["t2"][:], E["L"][:], E["lw"][:], ALU.subtract, [Et["L"], Et["lw"]], [Et["t2"]])
            _act(P, E["eLp"][:], E["t2"][:], AF.Exp, [Et["t2"]], [Et["eLp"]])
            _tt(P, 'dve', E["t1"][:].rearrange("p (c t) -> p c t", t=64), L3[:, :, 63:64].to_broadcast([128, NC, 64]), L3,
                ALU.subtract, [Et["L"]], [Et["t1"]])
            _act(P, E["eC"][:], E["t1"][:], AF.Exp, [Et["t1"]], [Et["eC"]])
            _act(P, gC[:], E["L"][:, 63:NT:64], AF.Exp, [Et["L"]], [gC_t])
            if STOP == 22: break
            v3 = lambda t: t[:].rearrange("p (c t) -> p c t", t=64)
            _stt(P, 'dve', AR[:, :, 0, :], v3(E["kk"]), -1.0, v3(E["eLp"]), ALU.mult, ALU.mult, [Et["kk"], Et["eLp"]], [AR_t])
            _tt(P, 'pool', AR[:, :, 1, :], v3(r), v3(E["eL"]), ALU.mult, [r_t, Et["eL"]], [AR_t])
            _tt(P, 'dve', E["BT"][:], E["bb"][:], E["eLm"][:], ALU.mult, [Et["bb"], Et["eLm"]], [Et["BT"]])
            _tt(P, 'pool', E["KTt"][:], E["kp"][:], E["eLm"][:], ALU.mult, [Et["kp"], Et["eLm"]], [Et["KTt"]])
            _tt(P, 'dve', E["KH"][:], E["kp"][:], E["eC"][:], ALU.mult, [Et["kp"], Et["eC"]], [Et["KH"]])
            _tt(P, 'pool', E["BH"][:], E["bb"][:], E["eC"][:], ALU.mult, [Et["bb"], Et["eC"]], [Et["BH"]])
            _stt(P, 'dve', E["t1"][:], r[:], pr(4), E["kp"][:], ALU.mult, ALU.mult, [r_t, Et["kp"], m_t], [Et["t1"]])
            ps, ps_t = PS.next()
            _mm(P, ps[:, 0:NT], bones[:], E["t1"][:], True, True, [k_t, Et["t1"]], [ps_t])
            _tt(P, 'dve', E["bonus"][:], ps[:, 0:NT], v[:], ALU.mult, [ps_t, v_t], [Et["bonus"]])
            if STOP == 23: break
            _cp(P, 'act', vb[:], v[:], [v_t], [vb_t])
            for qi, (n, src, src_t) in enumerate((("V", vb, vb_t), ("A", None, AR_t), ("KH", E["KH"], Et["KH"]), ("BH", E["BH"], Et["BH"]))):
                ps, ps_t = PSB.next()
                for c in range(NC):
                    in_ = AR[:, c, 0, :] if src is None else src[:, c * 64:(c + 1) * 64]
                    _tr(P, ps[0:64, c * 128:(c + 1) * 128], in_, C['idb'][:], [src_t, C['trk']], [ps_t])
                _cp(P, 'act' if qi % 2 else 'dve', TM[n][:], ps[0:64, 0:NC * 128].rearrange("p (c t) -> p c t", t=128), [ps_t], [TM_t[n]])
            if STOP == 24: break
            AR2 = AR[:].rearrange("p c a t -> p c (a t)")
            HS = [slice(0, 64), slice(64, 128)]
            for hh in range(2):
                hs = HS[hh]
                for pr2 in range(NC // 2):
                    ps, ps_t = PS.next()
                    for cc in range(2):
                        c = pr2 * 2 + cc
                        _mm(P, ps[0:64, cc * 256:cc * 256 + 128], E["BT"][hs, c * 64:(c + 1) * 64], AR2[hs, c, :], True, True,
                            [Et["BT"], AR_t], [ps_t])
                        _mm(P, ps[0:64, cc * 256 + 128:cc * 256 + 256], E["KTt"][hs, c * 64:(c + 1) * 64], AR2[hs, c, :], True, True,
                            [Et["KTt"], AR_t], [ps_t])
                    _tt(P, 'dve', M12[hh][:, pr2 * 2:pr2 * 2 + 2, :], ps[0:64, :].rearrange("p (c t) -> p c t", t=256), maskA[:],
                        ALU.mult, [ps_t, k_t], [M12_t[hh]])
            for hh in range(2):
                hs = HS[hh]
                ps, ps_t = PS.next()
                for c in range(NC):
                    _mm(P, ps[0:64, c * 64:(c + 1) * 64], AR[hs, c, 0, :], E["BT"][hs, c * 64:(c + 1) * 64], True, True, [AR_t, Et["BT"]], [ps_t])
                _tt(P, 'dve', PQ[hh][0][:, :, 0:64], ps[0:64, 0:NC * 64].rearrange("p (c t) -> p c t", t=64), maskP[:, 0:NC, :], ALU.mult,
                    [ps_t, k_t], [PQ_t[hh][0]])
                _cp(P, 'pool', PQ[hh][0][:, :, 64:128], M12[hh][:, :, 0:64], [M12_t[hh]], [PQ_t[hh][0]])
                _tt(P, 'pool', TT[hh][:], M12[hh][:, :, 0:64], id8[:, 0:NC, :], ALU.add, [M12_t[hh], k_t], [TT_t[hh]])
                _cp(P, 'act', M12b[:, hh, :, 0:64], M12[hh][:, :, 64:128], [M12_t[hh]], [M12b_t])
                _cp(P, 'act', M12b[:, hh, :, 64:128], M12[hh][:, :, 192:256], [M12_t[hh]], [M12b_t])
            cur = 0
            for lvl in range(5):
                nxt = 1 - cur
                last = lvl == 4
                for hh in range(2):
                    for half in range(NC // 4):
                        ps, ps_t = PS.next()
                        for cc in range(4):
                            c = half * 4 + cc
                            _mm(P, ps[0:64, cc * 128:cc * 128 + 64], PQ[hh][cur][:, c, 64:128], PQ[hh][cur][:, c, 0:64], True, True,
                                [PQ_t[hh][cur]], [ps_t])
                            if not last:
                                _mm(P, ps[0:64, cc * 128 + 64:cc * 128 + 128], PQ[hh][cur][:, c, 0:64], PQ[hh][cur][:, c, 64:128], True, True,
                                    [PQ_t[hh][cur]], [ps_t])
                        if last:
                            _cp(P, 'act', PQ[hh][nxt][:, half * 4:(half + 1) * 4, 0:64],
                                ps[0:64, :].rearrange("p (c t) -> p c t", t=128)[:, :, 0:64], [ps_t], [PQ_t[hh][nxt]])
                        else:
                            _cp(P, 'act' if (half + hh) % 2 else 'dve', PQ[hh][nxt][:, half * 4:(half + 1) * 4, :],
                                ps[0:64, :].rearrange("p (c t) -> p c t", t=128), [ps_t], [PQ_t[hh][nxt]])
                for hh in range(2):
                    ps, ps_t = PS.next()
                    for c in range(NC):
                        _mm(P, ps[0:64, c * 64:(c + 1) * 64], PQ[hh][nxt][:, c, 0:64], TT[hh][:, c, :], True, True,
                            [PQ_t[hh][nxt], TT_t[hh]], [ps_t])
                    _tt(P, 'dve', TT[hh][:], ps[0:64, 0:NC * 64].rearrange("p (c t) -> p c t", t=64), TT[hh][:], ALU.add,
                        [ps_t, TT_t[hh]], [TT_t[hh]])
                cur = nxt
            for hh in range(2):
                vc = HS[hh]
                ps, ps_t = PS.next()
                for c in range(NC):
                    _mm(P, ps[0:64, c * 64:(c + 1) * 64], M12[hh][:, c, 128:192], TM["V"][:, c, vc], True, True, [M12_t[hh], TM_t["V"]], [ps_t])
                _cp(P, 'act', Zs[hh][:], ps[0:64, 0:NC * 64].rearrange("p (c t) -> p c t", t=64), [ps_t], [Zs_t[hh]])
            for hh in range(2):
                hs = HS[hh]
                ps, ps_t = PS.next()
                for c in range(NC):
                    _mm(P, ps[:, c * 64:(c + 1) * 64], TM["A"][:, c, :], TT[hh][:, c, :], True, True, [TM_t["A"], TT_t[hh]], [ps_t])
                _cp(P, 'dve', W1T[hs, :, :], ps[hs, 0:NC * 64].rearrange("p (c t) -> p c t", t=64), [ps_t], [W1T_t])
            for hh in range(2):
                ps, ps_t = PS.next()
                for c in range(NC):
                    _mm(P, ps[0:64, c * 64:(c + 1) * 64], TT[hh][:, c, :], Zs[hh][:, c, :], True, True, [TT_t[hh], Zs_t[hh]], [ps_t])
                _cp(P, 'act', U0s[:, :, hh, :], ps[0:64, 0:NC * 64].rearrange("p (c t) -> p c t", t=64), [ps_t], [U0s_t])
            if STOP in (25, 26, 27): break
            for c in range(NC):
                ps, ps_t = PS.next()
                for hh in range(2):
                    hs = slice(hh * 64, (hh + 1) * 64)
                    _mm(P, ps[0:64, hh * 64:(hh + 1) * 64], W1T[hs, c, :], Hb[hp][hs, :], True, True, [W1T_t, Hb_t[hp]], [ps_t], force_own=(hh == 1))
                us, us_t = Us.next()
                _tt(P, 'dve', us[:], ps[0:64, 0:128], U0s[:, c, :, :].rearrange("p a t -> p (a t)"), ALU.add, [ps_t, U0s_t], [us_t])
                psy, psy_t = PS.next()
                for hh in range(2):
                    hs = slice(hh * 64, (hh + 1) * 64)
                    vc = slice(hh * 64, (hh + 1) * 64)
                    _mm(P, psy[0:64, vc], AR[hs, c, 1, :], Hb[hp][hs, :], True, False, [AR_t, Hb_t[hp]], [psy_t], force_own=(hh == 1))
                    _mm(P, psy[0:64, vc], M12b[:, hh, c, 0:64], us[:, vc], False, False, [M12b_t, us_t], [psy_t], force_own=(hh == 1))
                    _mm(P, psy[0:64, vc], M12b[:, hh, c, 64:128], TM["V"][:, c, vc], False, True, [M12b_t, TM_t["V"]], [psy_t])
                _cp(P, 'act', Ysb[:, c, :], psy[0:64, 0:128], [psy_t], [Ysb_t])
                psh, psh_t = PS.next()
                _mm(P, psh[:, 0:128], TM["KH"][:, c, :], TM["V"][:, c, :], True, False, [TM_t["KH"], TM_t["V"]], [psh_t])
                _mm(P, psh[:, 0:128], TM["BH"][:, c, :], us[:], False, True, [TM_t["BH"], us_t], [psh_t])
                for hh in range(2):
                    hs = slice(hh * 64, (hh + 1) * 64)
                    _stt(P, 'dve', Hs[hp][hs, :], Hs[hp][hs, :], gC[hs, c:c + 1], psh[hs, hh * 64:(hh + 1) * 64], ALU.mult, ALU.add,
                         [Hs_t[hp], gC_t, psh_t], [Hs_t[hp]])
                _cp(P, 'act', Hb[hp][:], Hs[hp][:], [Hs_t[hp]], [Hb_t[hp]])
            if STOP == 28: break
            Y4 = Ysb[:].rearrange("p c (a t) -> p (c a) t", t=64)
            P.op('dve', lambda e: e.tensor_reduce(out=st[:, 0, :], in_=Ysb[:].rearrange("p c (a t) -> p (c a) t", t=64), axis=AX.X, op=ALU.add),
                 [Ysb_t], [st_t])
            _act(P, Ysq[:], Ysb[:], AF.Square, [Ysb_t], [Ysq_t])
            P.op('dve', lambda e: e.tensor_reduce(out=st[:, 1, :], in_=Ysq[:].rearrange("p c (a t) -> p (c a) t", t=64), axis=AX.X, op=ALU.add),
                 [Ysq_t], [st_t])
            _ts(P, 'dve', st[:, 0, :], st[:, 0, :], 1.0 / 64, None, ALU.mult, None, [st_t], [st_t])
            _tt(P, 'dve', st[:, 2, :], st[:, 0, :], st[:, 0, :], ALU.mult, [st_t], [st_t])
            _stt(P, 'dve', st[:, 1, :], st[:, 1, :], 1.0 / 64, st[:, 2, :], ALU.mult, ALU.subtract, [st_t], [st_t])
            _ts(P, 'dve', st[:, 1, :], st[:, 1, :], 64e-5, None, ALU.add, None, [st_t], [st_t])
            _act(P, st[:, 1, :], st[:, 1, :], AF.Sqrt, [st_t], [st_t])
            _recip(P, st[:, 1, :], st[:, 1, :], [st_t], [st_t])
            Yq4 = Ysq[:].rearrange("p c (a t) -> p (c a) t", t=64)
            _tt(P, 'dve', Yq4, Y4, st[:, 0, :].unsqueeze(2).to_broadcast([64, NC * 2, 64]), ALU.subtract, [Ysb_t, st_t], [Ysq_t])
            _tt(P, 'dve', Yq4, Yq4, st[:, 1, :].unsqueeze(2).to_broadcast([64, NC * 2, 64]), ALU.mult, [Ysq_t, st_t], [Ysq_t])
            ps, ps_t = PS.next()
            for c in range(NC):
                _tr(P, ps[:, c * 64:(c + 1) * 64], Ysq[:, c, :], idf[0:64, 0:64], [Ysq_t, C['trk']], [ps_t])
            _ts(P, 'dve', E["YT"][:], ps[:, 0:NT], pr(5), pr(6), ALU.mult, ALU.add, [ps_t, m_t], [Et["YT"]])
            _tt(P, 'pool', E["YT"][:], E["YT"][:], E["bonus"][:], ALU.add, [Et["YT"], Et["bonus"]], [Et["YT"]])
            _tt(P, 'pool', E["YT"][:], E["YT"][:], E["g"][:], ALU.mult, [Et["YT"], Et["g"]], [Et["YT"]])
            P.dma('sp', 'yst', yT[hp * 128:(hp + 1) * 128, i * NT:(i + 1) * NT], E["YT"][:], reads=[Et["YT"]], writes=[out_t])
    return env.finish([out_t], 'sp')


def rwkv_inputs(xb, p, norm_mix, w_in, shift_mu, w0, w2, a0, a2, g2, kk, ka, rk, lnx_g, lnx_b):
    hs = slice(p * 256, (p + 1) * 256)
    cols = np.concatenate([np.arange(0, 512)[hs], 512 + np.arange(0, 512)[hs], 1024 + np.arange(0, 512)[hs], np.arange(1536, 1792)])
    W = w_in[:, cols]
    mu = shift_mu[cols]
    prm = np.zeros((128, 2, 8), np.float32)
    for j, a in enumerate((w0, a0, kk, ka, rk.reshape(-1), lnx_g, lnx_b)):
        prm[:, :, j] = a[hs].reshape(2, 128).T
    return {"x": xb, "gain": np.ascontiguousarray(norm_mix.reshape(8, 128).T), "W": np.ascontiguousarray(W),
            "mu": np.ascontiguousarray(mu.reshape(8, 128).T), "prm": prm, "w2": np.ascontiguousarray(w2[:, hs]),
            "a2": np.ascontiguousarray(a2[:, hs]), "g2": np.ascontiguousarray(g2[:, hs])}


def build_fused(S):
    nc = bass.Bass("TRN2", target_bir_lowering=False)
    P = Prog(nc)
    T = S // 2
    x = nc.dram_tensor("x", [S, D_MODEL], F32, kind="ExternalInput").ap()
    Y0 = nc.dram_tensor("Y0", [D_MODEL, S], F32).ap()
    H1 = nc.dram_tensor("H1", [S, D_MODEL], F32).ap()
    Y1 = nc.dram_tensor("Y1", [D_MODEL, S], F32).ap()
    out = nc.dram_tensor("out", [T, D_MODEL], F32, kind="ExternalOutput").ap()
    C = make_consts(P)
    dram_t = {"Y0": Trk(), "H1": Trk(), "Y1": Trk()}
    base = (nc.sbuf_base, nc.psum_base)

    def phase(fn, prefix, mapping, *args, **kw):
        env = Env(nc, P, C, mapping, prefix)
        fn(*args, env=env, **kw)
        P.barrier()
        nc.sbuf_base, nc.psum_base = base

    for g in range(2):
        phase(build_rwkv, f"rw{g}_", {"x": x, "yT": Y0[g * 256:(g + 1) * 256, :]}, S)
    for g in range(2):
        phase(build_mla, f"ml{g}_", {"x": x, "yT": Y0[512 + g * 256:512 + (g + 1) * 256, :]}, S)
    phase(build_tok, "t0_", {"x": x, "yT": Y0, "out": H1}, S, False, 1, 2816)
    for g in range(2):
        phase(build_fox, f"fx{g}_", {"x": H1, "yT": Y1[g * 256:(g + 1) * 256, :]}, S)
    for g in range(2):
        phase(build_moba, f"mb{g}_", {"x": H1, "yT": Y1[512 + g * 256:512 + (g + 1) * 256, :]}, S)
    phase(build_tok, "t1_", {"x": H1, "yT": Y1, "out": out}, T, True, 8, 3584, sel=True)
    P.emit()
    return nc


def fox_inputs(xb, p, norm_mix, w_in, bf):
    hs = slice(p * 256, (p + 1) * 256)
    W = np.concatenate([w_in[:, 0:512][:, hs], w_in[:, 512:1024][:, hs], w_in[:, 1024:1536][:, hs],
                        w_in[:, 1536 + 4 * p:1536 + 4 * p + 4]], axis=1)
    return {"x": xb, "gain": np.ascontiguousarray(norm_mix.reshape(8, 128).T), "W": np.ascontiguousarray(W),
            "bf": np.ascontiguousarray(bf[4 * p:4 * p + 4].reshape(4, 1))}


def fused_inputs(f, b, p, S):
    m = {"x": np.ascontiguousarray(f['x'][b])}

    def add(prefix, d):
        for k, v in d.items():
            if k != "x":
                m[prefix + k] = v
    for g in range(2):
        add(f"rw{g}_", rwkv_inputs(None, g, f['norm_mix_0'], f['w_in_0'], f['shift_mu_0'], f['rw_w0_0'], f['rw_w2_0'], f['rw_a0_0'],
                                   f['rw_a2_0'], f['rw_g2_0'], f['rw_kk_0'], f['rw_ka_0'], f['rw_rk_0'], f['rw_lnx_g_0'], f['rw_lnx_b_0']))
        add(f"ml{g}_", mla_inputs(None, g, f['norm_mix_0'], f['w_in_0'], f['mla_qnorm_0'], f['mla_wuq_0'], f['mla_kvnorm_0'],
                                  f['mla_wukv_0'], S))
        add(f"fx{g}_", fox_inputs(None, g, f['norm_mix_1'], f['w_in_1'], f['fox_bf_1']))
        add(f"mb{g}_", moba_inputs(None, g, f['norm_mix_1'], f['w_in_1'], S))
    add("t0_", {"Wout": f['w_out_0'], "gff": f['norm_ffn_0'].reshape(1, -1), "Wg": f['ffn_wg_0'][None], "Wu": f['ffn_wu_0'][None],
                "Wd": f['ffn_wd_0'][None]})
    add("t1_", {"Wout": f['w_out_1'], "gff": f['norm_ffn_1'].reshape(1, -1), "Wg": f['moe_wg_1'], "Wu": f['moe_wu_1'],
                "Wd": f['moe_wd_1'], "router": f['router_1'], "fgain": f['final_norm'].reshape(1, -1),
                "psel": np.ascontiguousarray(np.broadcast_to(np.array([1.0 - p, float(p)], np.float32), (128, 2)))})
    return m


_NC_CACHE = {}


def kernel(**inp):
    f = {k: np.ascontiguousarray(np.asarray(v, dtype=np.float32)) for k, v in inp.items()}
    B, S, D = f['x'].shape
    if S not in _NC_CACHE:
        _NC_CACHE[S] = build_fused(S)
    nc = _NC_CACHE[S]
    cores = [(b, p) for b in range(B) for p in range(2)]
    maps = [fused_inputs(f, b, p, S) for (b, p) in cores]
    res = run_bass_kernel_spmd(nc, maps, core_ids=list(range(len(cores)))).results
    out = np.stack([np.concatenate([res[2 * b]["out"], res[2 * b + 1]["out"]], axis=0) for b in range(B)], axis=0)
    return out.astype(np.float32)
```

```python
import numpy as np
import concourse.bass as bass
import concourse.mybir as mybir
from concourse.bass_utils import run_bass_kernel_spmd

F32 = mybir.dt.float32
BF16 = mybir.dt.bfloat16
AF = mybir.ActivationFunctionType
ALU = mybir.AluOpType
AX = mybir.AxisListType

ENGS = ('pe', 'act', 'dve', 'pool', 'sp')


class Trk:
    __slots__ = ('w', 'r', 'excl')

    def __init__(self, excl=False):
        self.w = None
        self.r = {}
        self.excl = excl


class Prog:
    def __init__(self, nc):
        self.nc = nc
        self.sem = {e: nc.alloc_semaphore("sem_" + e) for e in ENGS if e != 'sp'}
        self.cnt = {e: 0 for e in ENGS}
        self.waited = {e: {} for e in ENGS}
        self.ops = {e: [] for e in ENGS}
        self.dsem = {}
        self.dcnt = {}
        self.nbuf = 0
        self.same_engine_sync = True
        self.prefix = ""

    def sb(self, shape, dtype, name=None):
        self.nbuf += 1
        return self.nc.alloc_sbuf_tensor(self.prefix + (name or f"sb{self.nbuf}"), list(shape), dtype)

    def ps(self, shape, dtype=F32, name=None):
        self.nbuf += 1
        return self.nc.alloc_psum_tensor(self.prefix + (name or f"ps{self.nbuf}"), list(shape), dtype)

    def dma_sem(self, name):
        if name not in self.dsem:
            self.dsem[name] = self.nc.alloc_semaphore("dsem_" + name)
            self.dcnt[name] = 0
        return name

    def _waits(self, eng, reads, writes, force_own=False):
        need = {}
        for t in reads:
            if t.w is not None:
                s, v = t.w
                if need.get(s, 0) < v:
                    need[s] = v
            if t.excl:
                for s, v in t.r.items():
                    if need.get(s, 0) < v:
                        need[s] = v
        for t in writes:
            if t.w is not None:
                s, v = t.w
                if need.get(s, 0) < v:
                    need[s] = v
            for s, v in t.r.items():
                if need.get(s, 0) < v:
                    need[s] = v
        own = self.sem.get(eng)
        for s, v in need.items():
            if self.waited[eng].get(s, 0) >= v:
                continue
            if s is own and (eng == 'pe' or not self.same_engine_sync) and not force_own:
                continue
            self.ops[eng].append(('wait', s, v))
            self.waited[eng][s] = v

    def op(self, eng, fn, reads=(), writes=(), force_own=False):
        self._waits(eng, reads, writes, force_own)
        self.cnt[eng] += 1
        tag = (self.sem[eng], self.cnt[eng])
        self.ops[eng].append(('ins', fn, tag[0], 1))
        for t in reads:
            if t.r.get(tag[0], 0) < tag[1]:
                t.r[tag[0]] = tag[1]
        for t in writes:
            t.w = tag
            t.r = {}
        return tag

    def dma(self, q, semname, out, in_, reads=(), writes=(), **kw):
        self.dma_sem(semname)
        self._waits(q, reads, writes)
        self.dcnt[semname] += 16
        s = self.dsem[semname]
        tag = (s, self.dcnt[semname])
        self.ops[q].append(('ins', lambda e: e.dma_start(out=out, in_=in_, **kw), s, 16))
        for t in reads:
            if t.r.get(s, 0) < tag[1]:
                t.r[s] = tag[1]
        for t in writes:
            t.w = tag
            t.r = {}
        return tag

    def barrier(self):
        for e in ENGS:
            for e2, sm in self.sem.items():
                v = self.cnt[e2]
                if v > 0 and self.waited[e].get(sm, 0) < v:
                    self.ops[e].append(('wait', sm, v))
                    self.waited[e][sm] = v
            for name, sm in self.dsem.items():
                v = self.dcnt[name]
                if v > 0 and self.waited[e].get(sm, 0) < v:
                    self.ops[e].append(('wait', sm, v))
                    self.waited[e][sm] = v

    def wait_all(self, eng, trks):
        self._waits(eng, (), trks)

    def emit(self):
        nc = self.nc
        ops = self.ops

        def run(e, lst):
            for o in lst:
                if o[0] == 'wait':
                    e.wait_ge(o[1], o[2])
                else:
                    o[1](e).then_inc(o[2], o[3])

        with nc.Block() as block:
            @block.tensor
            def _(e):
                run(e, ops['pe'])

            @block.scalar
            def _(e):
                run(e, ops['act'])

            @block.vector
            def _(e):
                run(e, ops['dve'])

            @block.gpsimd
            def _(e):
                run(e, ops['pool'])

            @block.sync
            def _(e):
                run(e, ops['sp'])


def _mm(P, out, lhsT, rhs, start, stop, reads, writes, force_own=False):
    return P.op('pe', lambda e: e.matmul(out, lhsT=lhsT, rhs=rhs, start=start, stop=stop), reads, writes, force_own)


def _tr(P, out, in_, ident, reads, writes):
    return P.op('pe', lambda e: e.transpose(out, in_, ident), reads, writes)


def _act(P, out, in_, func, reads, writes, bias=None, scale=None, accum=None):
    kw = {}
    if bias is not None:
        kw['bias'] = bias
    if scale is not None:
        kw['scale'] = scale
    if accum is not None:
        kw['accum_out'] = accum
    return P.op('act', lambda e: e.activation(out=out, in_=in_, func=func, **kw), reads, writes)


def _tt(P, eng, out, in0, in1, op, reads, writes):
    return P.op(eng, lambda e: e.tensor_tensor(out=out, in0=in0, in1=in1, op=op), reads, writes)


def _ts(P, eng, out, in0, s1, s2, op0, op1, reads, writes, accum=None):
    if s2 is None:
        return P.op(eng, lambda e: e.tensor_scalar(out=out, in0=in0, scalar1=s1, scalar2=None, op0=op0), reads, writes)
    if accum is not None:
        return P.op(eng, lambda e: e.tensor_scalar(out=out, in0=in0, scalar1=s1, scalar2=s2, op0=op0, op1=op1, accum_out=accum), reads, writes)
    return P.op(eng, lambda e: e.tensor_scalar(out=out, in0=in0, scalar1=s1, scalar2=s2, op0=op0, op1=op1), reads, writes)


def _stt(P, eng, out, in0, scalar, in1, op0, op1, reads, writes):
    return P.op(eng, lambda e: e.scalar_tensor_tensor(out=out, in0=in0, scalar=scalar, in1=in1, op0=op0, op1=op1), reads, writes)


def _cp(P, eng, out, in_, reads, writes):
    if eng == 'act':
        return P.op('act', lambda e: e.copy(out=out, in_=in_), reads, writes)
    return P.op(eng, lambda e: e.tensor_copy(out=out, in_=in_), reads, writes)


def _memset(P, eng, ap, val, writes):
    return P.op(eng, lambda e: e.memset(ap, val), (), writes)


def _recip(P, out, in_, reads, writes):
    return P.op('dve', lambda e: e.reciprocal(out=out, in_=in_), reads, writes)


class Ring:
    def __init__(self, P, n, shape, dtype, psum=False, name=None):
        self.n = n
        self.bufs = []
        for j in range(n):
            t = (P.ps if psum else P.sb)(shape, dtype, name=(f"{name}_{j}" if name else None))
            self.bufs.append((t, Trk(excl=psum)))
        self.k = 0

    def next(self):
        b = self.bufs[self.k % self.n]
        self.k += 1
        return b


def make_consts(P):
    c = {}
    t = Trk()
    idb = P.sb([128, 128], BF16, "identb")
    idf = P.sb([128, 128], F32, "identf")
    tri = P.sb([128, 128], BF16, "tri")
    ones = P.sb([128, 128], BF16, "onesb")
    onesf = P.sb([128, 128], F32, "onesf")
    _memset(P, 'pool', idf[:], 1.0, [t])
    P.op('pool', lambda e: e.affine_select(out=idf[:], in_=idf[:], pattern=[[-1, 128]], compare_op=ALU.is_equal,
                                           fill=0.0, base=0, channel_multiplier=1), [t], [t])
    _cp(P, 'pool', idb[:], idf[:], [t], [t])
    _memset(P, 'pool', tri[:], 1.0, [t])
    P.op('pool', lambda e: e.affine_select(out=tri[:], in_=tri[:], pattern=[[1, 128]], compare_op=ALU.is_ge,
                                           fill=0.0, base=0, channel_multiplier=-1), [t], [t])
    _memset(P, 'pool', ones[:], 1.0, [t])
    _memset(P, 'pool', onesf[:], 1.0, [t])
    c.update(idb=idb, idf=idf, tri=tri, ones=ones, onesf=onesf, trk=t)
    return c


def load_w(P, W, K, N, gain=None, gtrk=None, name="w", stage=None, neg_cols=None):
    kc = K // 128
    wb = P.sb([128, kc, N], BF16, name)
    trk = Trk()
    if stage is None:
        stage = Ring(P, 2, [128, N], F32, name=name + "_st")
    for c in range(kc):
        st, strk = stage.next()
        P.dma('sp', f"{name}_st{(stage.k - 1) % stage.n}", st[:, 0:N], W[c * 128:(c + 1) * 128, :], writes=[strk])
        eng = 'act' if c % 2 == 0 else 'dve'
        if gain is not None:
            if eng == 'act':
                _act(P, wb[:, c, :], st[:, 0:N], AF.Copy, [strk, gtrk], [trk], scale=gain[:, c:c + 1])
            else:
                _ts(P, 'dve', wb[:, c, :], st[:, 0:N], gain[:, c:c + 1], None, ALU.mult, None, [strk, gtrk], [trk])
        else:
            _cp(P, eng, wb[:, c, :], st[:, 0:N], [strk], [trk])
        if neg_cols is not None:
            for (a, b) in neg_cols:
                _ts(P, 'dve', wb[:, c, a:b], wb[:, c, a:b], -1.0, None, ALU.mult, None, [trk], [trk])
    return wb, trk


class Front:
    def __init__(self, P, C, x, NT, eps=1e-6, D=1024, name="fe", n_uT=2, n_xs=3, n_pt=2):
        self.P, self.C, self.x, self.NT, self.eps, self.D = P, C, x, NT, eps, D
        self.kc = D // 128
        self.xs = Ring(P, n_xs, [128, D], F32, name=name + "_xs")
        self.n_xs = n_xs
        self.u = Ring(P, 2, [128, D], BF16, name=name + "_u")
        self.junk = P.sb([128, D], BF16, name + "_junk")
        self.junk_t = Trk()
        self.ss = Ring(P, 4, [128, 1], F32, name=name + "_ss")
        self.pt = Ring(P, n_pt, [128, D], BF16, psum=True, name=name + "_pt")
        self.uT = Ring(P, n_uT, [128, self.kc, NT], BF16, name=name + "_uT")
        self.name = name
        self.nev = 0

    def tile(self, i):
        P, C, NT, D = self.P, self.C, self.NT, self.D
        uT, uT_t = self.uT.next()
        for blk in range(NT // 128):
            r0 = i * NT + blk * 128
            xs, xs_t = self.xs.next()
            P.dma('sp', f"{self.name}_xs{(self.xs.k - 1) % self.n_xs}", xs[:], self.x[r0:r0 + 128, :], writes=[xs_t])
            ss, ss_t = self.ss.next()
            _act(P, self.junk[:], xs[:], AF.Square, [xs_t], [self.junk_t, ss_t], accum=ss[:])
            _ts(P, 'dve', ss[:], ss[:], 1.0 / D, self.eps, ALU.mult, ALU.add, [ss_t], [ss_t])
            _act(P, ss[:], ss[:], AF.Sqrt, [ss_t], [ss_t])
            _recip(P, ss[:], ss[:], [ss_t], [ss_t])
            u, u_t = self.u.next()
            _act(P, u[:], xs[:], AF.Copy, [xs_t, ss_t], [u_t], scale=ss[:, 0:1])
            pt, pt_t = self.pt.next()
            for c in range(self.kc):
                _tr(P, pt[:, c * 128:(c + 1) * 128], u[:, c * 128:(c + 1) * 128], C['idb'][:], [u_t, C['trk']], [pt_t])
            eng = 'dve' if self.nev % 2 == 0 else 'act'
            self.nev += 1
            _cp(P, eng, uT[:, :, blk * 128:(blk + 1) * 128], pt[:].rearrange("p (c t) -> p c t", t=128), [pt_t], [uT_t])
        return uT, uT_t


def proj_fm(P, ps, ps_t, W, W_t, c0, M, uT, uT_t, N, kc=8, n0=0):
    for c in range(kc):
        _mm(P, ps[0:M, 0:N], W[:, c, c0:c0 + M], uT[:, c, n0:n0 + N], c == 0, c == kc - 1, [W_t, uT_t], [ps_t])


def proj_tm(P, ps, ps_t, W, W_t, c0, ncols, uT, uT_t, t0, kc=8):
    for c in range(kc):
        _mm(P, ps[:, 0:ncols], uT[:, c, t0:t0 + 128], W[:, c, c0:c0 + ncols], c == 0, c == kc - 1, [W_t, uT_t], [ps_t])


class Attn:
    def __init__(self, P, C, NT, name="at", fox=False, n_pss=3, depth=1):
        self.P, self.C, self.NT = P, C, NT
        self.pss = Ring(P, n_pss, [128, NT], F32, psum=True, name=name + "_pss")
        self.depth = depth
        self.pT = Ring(P, 3, [128, NT], BF16, name=name + "_pT")
        self.po = Ring(P, 2, [128, NT], F32, psum=True, name=name + "_po")
        self.rden = Ring(P, 2, [128, NT], F32, name=name + "_rden")
        self.bcs = Ring(P, 2, [64, NT], F32, name=name + "_bcs")
        self.yo = Ring(P, 2, [64, NT], F32, name=name + "_yo")
        if fox:
            self.comb = Ring(P, 2, [65, NT], F32, name=name + "_comb")
            self.p2s = Ring(P, 1, [65, NT], F32, name=name + "_p2s")
        self.name = name
        self.nst = 0

    def head(self, i, KT, KT_trk, kp0, dk, QT, QT_t, Vtm, V_trk, vh, scale, y_dram, row0, fox=None, bg=None):
        P, C, NT = self.P, self.C, self.NT
        nq = NT // 128
        nkb = (i + 1) * nq
        ndiag0 = i * nq
        po, po_t = self.po.next()
        if fox is not None:
            po2, po2_t = self.po.next()

        def emit_S(kb):
            j = kb // nq
            off = max(0, kb - ndiag0) * 128
            ps, ps_t = self.pss.next()
            _mm(P, ps[:, off:NT], KT[kp0:kp0 + dk, kb * 128:(kb + 1) * 128], QT[kp0:kp0 + dk, off:NT], True, True,
                [KT_trk[j], QT_t], [ps_t])
            return ps, ps_t

        pend = [emit_S(kb) for kb in range(min(self.depth, nkb))]
        for kb in range(nkb):
            if kb + self.depth < nkb:
                pend.append(emit_S(kb + self.depth))
            ps, ps_t = pend.pop(0)
            if bg is not None:
                bg()
            j = kb // nq
            off = max(0, kb - ndiag0) * 128
            diag = kb >= ndiag0
            pT, pT_t = self.pT.next()
            if fox is None:
                _act(P, pT[:, off:NT], ps[:, off:NT], AF.Exp, [ps_t], [pT_t], scale=scale)
            elif not diag:
                _act(P, pT[:, off:NT], ps[:, off:NT], AF.Exp, [ps_t, fox['bt_t']], [pT_t], scale=scale, bias=fox['bt'][:, 0, kb:kb + 1])
            else:
                for qb in range(off // 128, nq):
                    _act(P, pT[:, qb * 128:(qb + 1) * 128], ps[:, qb * 128:(qb + 1) * 128], AF.Exp, [ps_t, fox['bt_t']], [pT_t],
                         scale=scale, bias=fox['bt'][:, qb, kb:kb + 1])
            if diag:
                eng = 'pool' if self.nst % 2 == 0 else 'dve'
                self.nst += 1
                _tt(P, eng, pT[:, off:off + 128], pT[:, off:off + 128], C['tri'][:], ALU.mult, [pT_t, C['trk']], [pT_t])
            if fox is not None and diag:
                _mm(P, po2[0:65, off:NT], Vtm[:, kb, vh, 0:65], pT[:, off:NT], kb == ndiag0, kb == nkb - 1, [V_trk[j], pT_t], [po2_t])
            elif fox is not None:
                _mm(P, po[0:65, off:NT], Vtm[:, kb, vh, 0:65], pT[:, off:NT], kb == 0, kb == ndiag0 - 1, [V_trk[j], pT_t], [po_t])
            else:
                _mm(P, po[0:65, off:NT], Vtm[:, kb, vh, 0:65], pT[:, off:NT], kb == 0, kb == nkb - 1, [V_trk[j], pT_t], [po_t])
        src, src_t = po, po_t
        if fox is not None:
            if i == 0:
                src, src_t = po2, po2_t
            else:
                p2s, p2s_t = self.p2s.next()
                _cp(P, 'act', p2s[:], po2[0:65, :], [po2_t], [p2s_t])
                cm, cm_t = self.comb.next()
                for qb in range(nq):
                    cs_ = slice(qb * 128, (qb + 1) * 128)
                    _stt(P, 'dve', cm[:, cs_], po[0:65, cs_], fox['f'][0:65, qb:qb + 1], p2s[:, cs_], ALU.mult, ALU.add,
                         [po_t, p2s_t, fox['f_t']], [cm_t])
                src, src_t = cm, cm_t
        rden, rden_t = self.rden.next()
        _recip(P, rden[64:65, :], src[64:65, :], [src_t], [rden_t])
        ps, ps_t = self.pss.next()
        P.op('pe', lambda e: e.matmul(ps[0:64, :], lhsT=C['onesf'][64:65, 0:64], rhs=rden[64:65, :], start=True, stop=True),
             [rden_t, C['trk']], [ps_t])
        bcs, bcs_t = self.bcs.next()
        _cp(P, 'act', bcs[:], ps[0:64, :], [ps_t], [bcs_t])
        yo, yo_t = self.yo.next()
        _tt(P, 'dve', yo[:], src[0:64, :], bcs[:], ALU.mult, [src_t, bcs_t], [yo_t])
        return yo, yo_t


class Env:
    def __init__(self, nc=None, P=None, C=None, mapping=None, prefix=""):
        self.fused = nc is not None
        self.nc = nc if nc is not None else bass.Bass("TRN2", target_bir_lowering=False)
        self.P = P if P is not None else Prog(self.nc)
        self.C = C
        self.mapping = mapping or {}
        self.prefix = prefix
        self.P.prefix = prefix

    def dram(self, name, shape, dtype, kind):
        if name in self.mapping:
            return self.mapping[name]
        return self.nc.dram_tensor(self.prefix + name, list(shape), dtype, kind=kind).ap()

    def consts(self):
        if self.C is None:
            self.C = make_consts(self.P)
        return self.C

    def finish(self, out_trks, eng):
        if self.fused:
            return self.nc
        self.P.wait_all(eng, out_trks)
        self.P.emit()
        return self.nc


D_MODEL = 1024
STOP = 0


def build_fox(S, NT=512, env=None):
    env = env or Env()
    nc = env.nc
    x = env.dram("x", [S, D_MODEL], F32, "ExternalInput")
    gain = env.dram("gain", [128, 8], F32, "ExternalInput")
    W = env.dram("W", [D_MODEL, 772], F32, "ExternalInput")
    bf = env.dram("bf", [4, 1], F32, "ExternalInput")
    yT = env.dram("yT", [256, S], F32, "ExternalOutput")
    P = env.P
    C = env.consts()
    nq = NT // 128
    ntile = S // NT
    nkb = S // 128
    g_sb = P.sb([128, 8], F32, "gain_sb"); g_t = Trk()
    P.dma('sp', 'misc', g_sb[:], gain, writes=[g_t])
    bf_sb = P.sb([4, 1], F32, "bf_sb"); bf_t = Trk()
    P.dma('sp', 'misc', bf_sb[:], bf, writes=[bf_t])
    Wb, W_t = load_w(P, W, D_MODEL, 772, g_sb, g_t, "Wb")
    fe = Front(P, C, x, NT, n_pt=1, n_xs=2)
    at = Attn(P, C, NT, fox=True)
    pp = Ring(P, 2, [128, NT], F32, psum=True, name="pp")
    KT = [P.sb([128, S], BF16, f"KT{hp}") for hp in range(2)]
    KT_trk = [[Trk() for _ in range(ntile)] for hp in range(2)]
    QT = Ring(P, 4, [128, NT], BF16, name="QT")
    Vtm = P.sb([128, nkb, 4, 65], BF16, "Vtm")
    V_trk = [Trk() for _ in range(ntile)]
    for j in range(ntile):
        _memset(P, 'pool', Vtm[:, j * nq:(j + 1) * nq, :, 64:65], 1.0, [V_trk[j]])
    Dt = Ring(P, 2, [4, NT], F32, name="Dt")
    lf = P.sb([4, NT], F32, "lf"); lf_t = Trk()
    onesrow = P.sb([4, NT], F32, "onesrow"); onesrow_t = Trk()
    _memset(P, 'pool', onesrow[:], 1.0, [onesrow_t])
    Dtm = P.sb([128, nkb, 4], F32, "Dtm")
    Dtm_trk = [Trk() for _ in range(ntile)]
    sel = P.sb([4, 4, 128], F32, "sel"); sel_t = Trk()
    for h in range(4):
        _cp(P, 'pool', sel[0:4, h, :], C['idf'][0:4, h:h + 1].to_broadcast([4, 128]), [C['trk']], [sel_t])
    cbc = Ring(P, 2, [128, 16], F32, name="cbc")
    biasT = Ring(P, 2, [128, nq, nkb], F32, name="biasT")
    fcol = Ring(P, 2, [128, nq], F32, name="fcol")
    out_t = Trk()
    prevD = None
    for i in range(ntile):
        uT, uT_t = fe.tile(i)
        qts = []
        for hp in range(2):
            ps, ps_t = pp.next()
            proj_fm(P, ps, ps_t, Wb, W_t, hp * 128, 128, uT, uT_t, NT)
            q, q_t = QT.next()
            _cp(P, 'act', q[:], ps[:], [ps_t], [q_t])
            qts.append((q, q_t))
            ps, ps_t = pp.next()
            proj_fm(P, ps, ps_t, Wb, W_t, 256 + hp * 128, 128, uT, uT_t, NT)
            _cp(P, 'dve', KT[hp][:, i * NT:(i + 1) * NT], ps[:], [ps_t], [KT_trk[hp][i]])
        for blk in range(nq):
            ps, ps_t = pp.next()
            proj_tm(P, ps, ps_t, Wb, W_t, 512, 256, uT, uT_t, blk * 128)
            _cp(P, 'act' if blk % 2 else 'dve', Vtm[:, i * nq + blk, :, 0:64], ps[:, 0:256].rearrange("p (h d) -> p h d", d=64),
                [ps_t], [V_trk[i]])
        ps, ps_t = pp.next()
        proj_fm(P, ps, ps_t, Wb, W_t, 768, 4, uT, uT_t, NT)
        _act(P, lf[:], ps[0:4, :], AF.Sigmoid, [ps_t, bf_t], [lf_t], bias=bf_sb[:, 0:1])
        _act(P, lf[:], lf[:], AF.Ln, [lf_t], [lf_t])
        D, D_t = Dt.next()
        if prevD is None:
            P.op('dve', lambda e, D=D: e.tensor_tensor_scan(out=D[:], data0=onesrow[:], data1=lf[:], initial=0.0,
                                                            op0=ALU.mult, op1=ALU.add), [onesrow_t, lf_t], [D_t])
        else:
            pD, pD_t = prevD
            P.op('dve', lambda e, D=D, pD=pD: e.tensor_tensor_scan(out=D[:], data0=onesrow[:], data1=lf[:],
                                                                   initial=pD[:, NT - 1:NT], op0=ALU.mult, op1=ALU.add),
                 [onesrow_t, lf_t, pD_t], [D_t])
        prevD = (D, D_t)
        for blk in range(nq):
            ps, ps_t = pp.next()
            _tr(P, ps[:, 0:4], D[0:4, blk * 128:(blk + 1) * 128], C['idf'][0:4, 0:4], [D_t, C['trk']], [ps_t])
            _cp(P, 'dve', Dtm[:, i * nq + blk, :], ps[:, 0:4], [ps_t], [Dtm_trk[i]])
        ps, ps_t = pp.next()
        for h in range(4):
            _mm(P, ps[:, h * nq:(h + 1) * nq], sel[0:4, h, :], D[0:4, 0:NT:128], True, True, [sel_t, D_t], [ps_t])
        cb, cb_t = cbc.next()
        _cp(P, 'act', cb[:, 0:4 * nq], ps[:, 0:4 * nq], [ps_t], [cb_t])
        nk = (i + 1) * nq
        for h in range(4):
            bt, bt_t = biasT.next()
            for qb in range(nq):
                _ts(P, 'pool', bt[:, qb, 0:nk], Dtm[:, 0:nk, h], -1.0, cb[:, h * nq + qb:h * nq + qb + 1], ALU.mult, ALU.add,
                    [cb_t] + Dtm_trk[0:i + 1], [bt_t])
            hp, hh = h // 2, h % 2
            q, q_t = qts[hp]
            fc, fc_t = fcol.next()
            _ts(P, 'dve', fc[:, 0:nq], cb[:, h * nq:(h + 1) * nq], cb[:, h * nq:h * nq + 1], None, ALU.subtract, None, [cb_t], [fc_t])
            _act(P, fc[:, 0:nq], fc[:, 0:nq], AF.Exp, [fc_t], [fc_t])
            yo, yo_t = at.head(i, KT[hp], KT_trk[hp], hh * 64, 64, q, q_t, Vtm, V_trk, h, 0.125, None, 0,
                               fox=dict(bt=bt, bt_t=bt_t, f=fc, f_t=fc_t))
            P.dma('pool', 'yst', yT[h * 64:(h + 1) * 64, i * NT:(i + 1) * NT], yo[:], reads=[yo_t], writes=[out_t])
    return env.finish([out_t], 'pool')


def build_mla(S, NT=512, env=None):
    env = env or Env()
    nc = env.nc
    x = env.dram("x", [S, D_MODEL], F32, "ExternalInput")
    gain = env.dram("gain", [128, 8], F32, "ExternalInput")
    W = env.dram("W", [D_MODEL, 576], F32, "ExternalInput")
    qg = env.dram("qg", [128, 2], F32, "ExternalInput")
    kvg = env.dram("kvg", [128, 1], F32, "ExternalInput")
    Wuq = env.dram("Wuq", [256, 768], F32, "ExternalInput")
    Wkv = env.dram("Wkv", [128, 512], F32, "ExternalInput")
    cs = env.dram("cs", [2, 32, S], F32, "ExternalInput")
    yT = env.dram("yT", [256, S], F32, "ExternalOutput")
    P = env.P
    C = env.consts()
    nq = NT // 128
    ntile = S // NT
    nkb = S // 128
    g_sb = P.sb([128, 8], F32, "gain_sb"); g_t = Trk()
    P.dma('sp', 'misc', g_sb[:], gain, writes=[g_t])
    qg_sb = P.sb([128, 2], F32, "qg_sb")
    P.dma('sp', 'misc', qg_sb[:], qg, writes=[g_t])
    kvg_sb = P.sb([128, 1], F32, "kvg_sb")
    P.dma('sp', 'misc', kvg_sb[:], kvg, writes=[g_t])
    stg = Ring(P, 2, [128, 768], F32, name="wstage")
    Wb, W_t = load_w(P, W, D_MODEL, 576, g_sb, g_t, "Wb", stage=stg, neg_cols=[(480 + 64, 480 + 64 + 16)])
    negq = [(384 + h * 96 + 64, 384 + h * 96 + 80) for h in range(4)]
    Wuqb, Wuq_t = load_w(P, Wuq, 256, 768, qg_sb, g_t, "Wuqb", stage=stg, neg_cols=negq)
    Wkvb, Wkv_t = load_w(P, Wkv, 128, 512, kvg_sb, g_t, "Wkvb", stage=stg)
    fe = Front(P, C, x, NT, n_xs=2, n_pt=1)
    at = Attn(P, C, NT, n_pss=4, depth=2)
    pp = Ring(P, 1, [128, NT], F32, psum=True, name="pp")
    KT = [P.sb([96, S], BF16, f"KT{h}") for h in range(4)]
    KT_trk = [[Trk() for _ in range(ntile)] for h in range(4)]
    QT = Ring(P, 8, [96, NT], BF16, name="QT")
    Vtm = P.sb([128, nkb, 4, 65], BF16, "Vtm")
    V_trk = [Trk() for _ in range(ntile)]
    for j in range(ntile):
        _memset(P, 'pool', Vtm[:, j * nq:(j + 1) * nq, :, 64:65], 1.0, [V_trk[j]])
    cst = Ring(P, 2, [96, 2, NT], F32, name="cst")
    qlb = Ring(P, 2, [128, 2, NT], BF16, name="qlb")
    kvb = Ring(P, 4, [128, NT], BF16, name="kvb")
    sq = Ring(P, 3, [128, NT], F32, name="sq")
    rq = Ring(P, 2, [128, NT], F32, name="rq")
    rkv = Ring(P, 2, [128, NT], F32, name="rkv")
    tmp = Ring(P, 3, [96, NT], F32, name="tmp")
    out_t = Trk()

    def rstd_bc(ps, ps_t, dst, dst_t, n):
        _ts(P, 'dve', dst[:], ps[:], 1.0 / n, 1e-6, ALU.mult, ALU.add, [ps_t], [dst_t])
        _act(P, dst[:], dst[:], AF.Sqrt, [dst_t], [dst_t])
        _recip(P, dst[:], dst[:], [dst_t], [dst_t])

    def prep(i):
        qs = []
        uT, uT_t = fe.tile(i)
        yield
        c_sb, c_t = cst.next()
        P.dma('sp', f"cs{(cst.k - 1) % 2}", c_sb[64:96, 0, :], cs[0, :, i * NT:(i + 1) * NT], writes=[c_t])
        P.dma('sp', f"cs{(cst.k - 1) % 2}", c_sb[64:96, 1, :], cs[1, :, i * NT:(i + 1) * NT], writes=[c_t])
        ql, ql_t = qlb.next()
        sqs = []
        for c in range(2):
            yield
            ps, ps_t = pp.next()
            proj_fm(P, ps, ps_t, Wb, W_t, c * 128, 128, uT, uT_t, NT)
            s_, s_t = sq.next()
            _act(P, s_[:], ps[:], AF.Square, [ps_t], [s_t])
            _cp(P, 'dve', ql[:, c, :], ps[:], [ps_t], [ql_t])
            sqs.append((s_, s_t))
        yield
        ps, ps_t = pp.next()
        for c in range(2):
            _mm(P, ps[:], C['onesf'][:], sqs[c][0][:], c == 0, c == 1, [C['trk'], sqs[c][1]], [ps_t])
        rq_sb, rq_t = rq.next()
        rstd_bc(ps, ps_t, rq_sb, rq_t, 256)
        yield
        ps, ps_t = pp.next()
        proj_fm(P, ps, ps_t, Wb, W_t, 256, 128, uT, uT_t, NT)
        s_, s_t = sq.next()
        _act(P, s_[:], ps[:], AF.Square, [ps_t], [s_t])
        kv, kv_t = kvb.next()
        _cp(P, 'dve', kv[:], ps[:], [ps_t], [kv_t])
        yield
        ps, ps_t = pp.next()
        _mm(P, ps[:], C['onesf'][:], s_[:], True, True, [C['trk'], s_t], [ps_t])
        rkv_sb, rkv_t = rkv.next()
        rstd_bc(ps, ps_t, rkv_sb, rkv_t, 128)
        kvn_, kvn_t = kvb.next()
        _tt(P, 'pool', kvn_[:], kv[:], rkv_sb[:], ALU.mult, [kv_t, rkv_t], [kvn_t])
        kv, kv_t = kvn_, kvn_t
        yield
        psa, psa_t = pp.next()
        proj_fm(P, psa, psa_t, Wb, W_t, 384, 96, uT, uT_t, NT)
        t1, t1_t = tmp.next()
        _tt(P, 'dve', t1[64:96, :], psa[64:96, :], c_sb[64:96, 0, :], ALU.mult, [psa_t, c_t], [t1_t])
        yield
        psb, psb_t = pp.next()
        proj_fm(P, psb, psb_t, Wb, W_t, 480, 96, uT, uT_t, NT)
        t2, t2_t = tmp.next()
        _tt(P, 'dve', t2[64:96, :], psb[64:96, :], c_sb[64:96, 1, :], ALU.mult, [psb_t, c_t], [t2_t])
        _tt(P, 'pool', t1[64:96, :], t1[64:96, :], t2[64:96, :], ALU.add, [t1_t, t2_t], [t1_t])
        for h in range(4):
            _cp(P, 'pool' if h % 2 else 'act', KT[h][64:96, i * NT:(i + 1) * NT], t1[64:96, :], [t1_t], [KT_trk[h][i]])
        for blk in range(nq):
            yield
            ps, ps_t = pp.next()
            _mm(P, ps[:, 0:256], kv[:, blk * 128:(blk + 1) * 128], Wkvb[:, 0, 256:512], True, True, [kv_t, Wkv_t], [ps_t])
            _cp(P, 'act' if blk % 2 else 'dve', Vtm[:, i * nq + blk, :, 0:64], ps[:, 0:256].rearrange("p (h d) -> p h d", d=64),
                [ps_t], [V_trk[i]])
        for h in range(4):
            yield
            ps, ps_t = pp.next()
            _mm(P, ps[0:64, :], Wkvb[:, 0, h * 64:(h + 1) * 64], kv[:], True, True, [kv_t, Wkv_t], [ps_t])
            _cp(P, 'act', KT[h][0:64, i * NT:(i + 1) * NT], ps[0:64, :], [ps_t], [KT_trk[h][i]])
            q, q_t = QT.next()
            yield
            psq, psq_t = pp.next()
            for c in range(2):
                _mm(P, psq[0:96, :], Wuqb[:, c, h * 96:(h + 1) * 96], ql[:, c, :], c == 0, c == 1, [Wuq_t, ql_t], [psq_t])
            _tt(P, 'dve', q[0:64, :], psq[0:64, :], rq_sb[0:64, :], ALU.mult, [psq_t, rq_t], [q_t])
            t1, t1_t = tmp.next()
            _tt(P, 'dve', t1[64:96, :], psq[64:96, :], c_sb[64:96, 0, :], ALU.mult, [psq_t, c_t], [t1_t])
            yield
            psr, psr_t = pp.next()
            for c in range(2):
                _mm(P, psr[0:96, :], Wuqb[:, c, 384 + h * 96:384 + (h + 1) * 96], ql[:, c, :], c == 0, c == 1, [Wuq_t, ql_t], [psr_t])
            t2, t2_t = tmp.next()
            _tt(P, 'dve', t2[64:96, :], psr[64:96, :], c_sb[64:96, 1, :], ALU.mult, [psr_t, c_t], [t2_t])
            _tt(P, 'pool', t1[64:96, :], t1[64:96, :], t2[64:96, :], ALU.add, [t1_t, t2_t], [t1_t])
            _tt(P, 'pool', q[64:96, :], t1[64:96, :], rq_sb[64:96, :], ALU.mult, [t1_t, rq_t], [q_t])
            qs.append((q, q_t))
            yield


        res[i] = qs

    res = {}

    def drain(g):
        for _ in g:
            pass

    drain(prep(0))
    for i in range(ntile):
        g = prep(i + 1) if i + 1 < ntile else None

        def bg(g=g):
            if g is not None:
                next(g, None)
        for h in range(4):
            q, q_t = res[i][h]
            yo, yo_t = at.head(i, KT[h], KT_trk[h], 0, 96, q, q_t, Vtm, V_trk, h, 96 ** -0.5, None, 0, bg=bg)
            P.dma('pool', 'yst', yT[h * 64:(h + 1) * 64, i * NT:(i + 1) * NT], yo[:], reads=[yo_t], writes=[out_t])
        if g is not None:
            drain(g)
        del res[i]
    return env.finish([out_t], 'pool')


def rope_tab(S, dim):
    inv = (10000.0 ** (-np.arange(0, dim, 2, dtype=np.float32) / np.float32(dim))).astype(np.float32)
    ang = np.arange(S, dtype=np.float32)[:, None] * inv[None, :]
    c = np.cos(ang).astype(np.float32).T
    s = np.sin(ang).astype(np.float32).T
    return np.ascontiguousarray(np.stack([np.concatenate([c, c], 0), np.concatenate([s, s], 0)], 0))


def mla_inputs(xb, p, norm_mix, w_in, qnorm, wuq, kvnorm, wukv, S):
    RW = 1792
    qlat = w_in[:, RW:RW + 256]
    kvlat = w_in[:, RW + 256:RW + 384]
    kpe = w_in[:, RW + 384:RW + 416]
    z64 = np.zeros((1024, 64), np.float32)
    perm = (np.arange(32) + 16) % 32
    W = np.concatenate([qlat, kvlat, z64, kpe, z64, kpe[:, perm]], axis=1)
    heads = range(4 * p, 4 * p + 4)
    uq = np.concatenate([wuq[:, h * 96:(h + 1) * 96] for h in heads], axis=1)
    z = np.zeros((256, 64), np.float32)
    uqr = np.concatenate([np.concatenate([z, wuq[:, h * 96 + 64:(h + 1) * 96][:, perm]], axis=1) for h in heads], axis=1)
    wk = np.concatenate([wukv[:, h * 128:h * 128 + 64] for h in heads], axis=1)
    wv = np.concatenate([wukv[:, h * 128 + 64:(h + 1) * 128] for h in heads], axis=1)
    return {"x": xb, "gain": np.ascontiguousarray(norm_mix.reshape(8, 128).T), "W": np.ascontiguousarray(W),
            "qg": np.ascontiguousarray(qnorm.reshape(2, 128).T), "kvg": np.ascontiguousarray(kvnorm.reshape(1, 128).T),
            "Wuq": np.ascontiguousarray(np.concatenate([uq, uqr], axis=1)),
            "Wkv": np.ascontiguousarray(np.concatenate([wk, wv], axis=1)), "cs": rope_tab(S, 32)}


def build_moba(S, NT=512, env=None):
    env = env or Env()
    nc = env.nc
    x = env.dram("x", [S, D_MODEL], F32, "ExternalInput")
    gain = env.dram("gain", [128, 8], F32, "ExternalInput")
    W = env.dram("W", [D_MODEL, 1280], F32, "ExternalInput")
    cs = env.dram("cs", [2, 64, S], F32, "ExternalInput")
    oh = env.dram("oh", [32, S], BF16, "ExternalInput")
    yT = env.dram("yT", [256, S], F32, "ExternalOutput")
    P = env.P
    C = env.consts()
    nq = NT // 128
    ntile = S // NT
    nkb = S // 128
    BIG = 30000.0
    g_sb = P.sb([128, 8], F32, "gain_sb"); g_t = Trk()
    P.dma('sp', 'misc', g_sb[:], gain, writes=[g_t])
    neg = [(768 + h * 64, 768 + h * 64 + 32) for h in range(4)] + [(1024 + h * 64, 1024 + h * 64 + 32) for h in range(4)]
    Wb, W_t = load_w(P, W, D_MODEL, 1280, g_sb, g_t, "Wb", neg_cols=neg)
    fe = Front(P, C, x, NT, n_pt=1)
    at = Attn(P, C, NT)
    pp = Ring(P, 2, [128, NT], F32, psum=True, name="pp")
    KT = [P.sb([96, S], BF16, f"KT{h}") for h in range(4)]
    KT_trk = [[Trk() for _ in range(ntile)] for h in range(4)]
    for h in range(4):
        for j in range(ntile):
            P.dma('sp', 'ohld', KT[h][64:96, j * NT:(j + 1) * NT], oh[:, j * NT:(j + 1) * NT], writes=[KT_trk[h][j]])
    QT = Ring(P, 2, [96, NT], BF16, name="QT")
    Vtm = P.sb([128, nkb, 4, 65], BF16, "Vtm")
    V_trk = [Trk() for _ in range(ntile)]
    for j in range(ntile):
        _memset(P, 'pool', Vtm[:, j * nq:(j + 1) * nq, :, 64:65], 1.0, [V_trk[j]])
    cst = Ring(P, 2, [64, 2, NT], F32, name="cst")
    tmp = Ring(P, 4, [64, NT], F32, name="tmp")
    kmT = P.sb([64, 4, max(S // 256, 8)], F32, "kmT"); km_t = [Trk() for h in range(4)]
    gate = Ring(P, 2, [128, 32], F32, name="gate")
    mx = Ring(P, 2, [128, 8], F32, name="mx")
    penpad = Ring(P, 2, [128, 96], F32, name="penpad")
    for (pb, pb_t) in penpad.bufs:
        _memset(P, 'pool', pb[:], 0.0, [pb_t])
    out_t = Trk()

    def roped(h, col0, rot0, uT, uT_t, c_sb, c_t):
        ps, ps_t = pp.next()
        proj_fm(P, ps, ps_t, Wb, W_t, col0 + h * 64, 64, uT, uT_t, NT)
        t1, t1_t = tmp.next()
        _tt(P, 'dve', t1[:], ps[0:64, :], c_sb[:, 0, :], ALU.mult, [ps_t, c_t], [t1_t])
        ps, ps_t = pp.next()
        proj_fm(P, ps, ps_t, Wb, W_t, rot0 + h * 64, 64, uT, uT_t, NT)
        t2, t2_t = tmp.next()
        _tt(P, 'dve', t2[:], ps[0:64, :], c_sb[:, 1, :], ALU.mult, [ps_t, c_t], [t2_t])
        _tt(P, 'pool', t1[:], t1[:], t2[:], ALU.add, [t1_t, t2_t], [t1_t])
        return t1, t1_t

    for i in range(ntile):
        uT, uT_t = fe.tile(i)
        c_sb, c_t = cst.next()
        P.dma('sp', f"cs{(cst.k - 1) % 2}", c_sb[:, 0, :], cs[0, :, i * NT:(i + 1) * NT], writes=[c_t])
        P.dma('sp', f"cs{(cst.k - 1) % 2}", c_sb[:, 1, :], cs[1, :, i * NT:(i + 1) * NT], writes=[c_t])
        for blk in range(nq):
            ps, ps_t = pp.next()
            proj_tm(P, ps, ps_t, Wb, W_t, 512, 256, uT, uT_t, blk * 128)
            _cp(P, 'act' if blk % 2 else 'dve', Vtm[:, i * nq + blk, :, 0:64], ps[:, 0:256].rearrange("p (h d) -> p h d", d=64),
                [ps_t], [V_trk[i]])
        for h in range(4):
            kf, kf_t = roped(h, 256, 1024, uT, uT_t, c_sb, c_t)
            _cp(P, 'act', KT[h][0:64, i * NT:(i + 1) * NT], kf[:], [kf_t], [KT_trk[h][i]])
            nb = NT // 256
            P.op('dve', lambda e, kf=kf, h=h, i=i, nb=nb: e.tensor_reduce(
                out=kmT[:, h, i * nb:(i + 1) * nb], in_=kf[:].rearrange("p (n t) -> p n t", t=256), axis=AX.X, op=ALU.add),
                [kf_t], [km_t[h]])
            _ts(P, 'dve', kmT[:, h, i * nb:(i + 1) * nb], kmT[:, h, i * nb:(i + 1) * nb], 1.0 / 256, None, ALU.mult, None,
                [km_t[h]], [km_t[h]])
            qf, qf_t = roped(h, 0, 768, uT, uT_t, c_sb, c_t)
            q, q_t = QT.next()
            _cp(P, 'act', q[0:64, :], qf[:], [qf_t], [q_t])
            for qb in range(nq):
                own = (i * NT + qb * 128) // 256
                pb, pb_t = penpad.next()
                if own >= 3:
                    ps, ps_t = pp.next()
                    _mm(P, ps[:, 0:own], qf[:, qb * 128:(qb + 1) * 128], kmT[:, h, 0:own], True, True, [qf_t, km_t[h]], [ps_t])
                    g_, g_t2 = gate.next()
                    _memset(P, 'pool', g_[:], -1e30, [g_t2])
                    _cp(P, 'dve', g_[:, 0:own], ps[:, 0:own], [ps_t], [g_t2])
                    m_, m_t = mx.next()
                    P.op('dve', lambda e, m_=m_, g_=g_: e.max(out=m_[:], in_=g_[:]), [g_t2], [m_t])
                    _ts(P, 'dve', g_[:], g_[:], m_[:, 2:3], None, ALU.is_ge, None, [g_t2, m_t], [g_t2])
                    _ts(P, 'dve', pb[:, 64:96], g_[:], -1.0, BIG, ALU.add, ALU.mult, [g_t2], [pb_t])
                    if own < 32:
                        _memset(P, 'dve', pb[:, 64 + own:96], 0.0, [pb_t])
                else:
                    _memset(P, 'dve', pb[:, 64:96], 0.0, [pb_t])
                ps, ps_t = pp.next()
                _tr(P, ps[0:96, 0:128], pb[:, 0:96], C['idf'][:], [pb_t, C['trk']], [ps_t])
                _cp(P, 'act', q[64:96, qb * 128:(qb + 1) * 128], ps[64:96, 0:128], [ps_t], [q_t])
            yo, yo_t = at.head(i, KT[h], KT_trk[h], 0, 96, q, q_t, Vtm, V_trk, h, 0.125, None, 0)
            P.dma('pool', 'yst', yT[h * 64:(h + 1) * 64, i * NT:(i + 1) * NT], yo[:], reads=[yo_t], writes=[out_t])
    return env.finish([out_t], 'pool')


def moba_inputs(xb, p, norm_mix, w_in, S):
    import ml_dtypes
    base = 1544
    hs = slice(p * 256, (p + 1) * 256)
    q = w_in[:, base:base + 512][:, hs]
    k = w_in[:, base + 512:base + 1024][:, hs]
    v = w_in[:, base + 1024:base + 1536][:, hs]
    perm = np.concatenate([h * 64 + (np.arange(64) + 32) % 64 for h in range(4)])
    W = np.concatenate([q, k, v, q[:, perm], k[:, perm]], axis=1)
    oh = np.zeros((32, S), np.float32)
    for n in range(min(32, S // 256)):
        oh[n, n * 256:(n + 1) * 256] = 1.0
    return {"x": xb, "gain": np.ascontiguousarray(norm_mix.reshape(8, 128).T), "W": np.ascontiguousarray(W),
            "cs": rope_tab(S, 64), "oh": oh.astype(ml_dtypes.bfloat16)}


def build_tok(T, moe, NE, DFF, TB=2048, FG=256, env=None, sel=False):
    env = env or Env()
    nc = env.nc
    TB = min(TB, T)
    TX = T * (2 if sel else 1)
    x = env.dram("x", [TX, D_MODEL], F32, "ExternalInput")
    yT = env.dram("yT", [D_MODEL, TX], F32, "ExternalInput")
    if sel:
        psel = env.dram("psel", [128, 2], F32, "ExternalInput")
    Wout = env.dram("Wout", [D_MODEL, D_MODEL], F32, "ExternalInput")
    gff = env.dram("gff", [1, D_MODEL], F32, "ExternalInput")
    Wg = env.dram("Wg", [NE, D_MODEL, DFF], F32, "ExternalInput")
    Wu = env.dram("Wu", [NE, D_MODEL, DFF], F32, "ExternalInput")
    Wd = env.dram("Wd", [NE, DFF, D_MODEL], F32, "ExternalInput")
    if moe:
        router = env.dram("router", [D_MODEL, 8], F32, "ExternalInput")
        fg_d = env.dram("fgain", [1, D_MODEL], F32, "ExternalInput")
    out = env.dram("out", [T, D_MODEL], F32, "ExternalOutput")
    P = env.P
    C = env.consts()
    D = D_MODEL
    nb = TB // 128
    nsub = TB // 512
    npass = T // TB
    ngrp = DFF // FG
    nj = FG // 128
    m_t = Trk()
    gbc = P.sb([128, D], F32, "gbc")
    P.dma('sp', 'misc', gbc[:], gff.partition_broadcast(128), writes=[m_t])
    if moe:
        fgbc = P.sb([128, D], F32, "fgbc")
        P.dma('sp', 'misc', fgbc[:], fg_d.partition_broadcast(128), writes=[m_t])
        rt_sb = P.sb([128, 8, 8], F32, "rt_sb")
        P.dma('sp', 'misc', rt_sb[:], router.rearrange("(c p) e -> p c e", p=128), writes=[m_t])
    if sel:
        psel_sb = P.sb([128, 2], F32, "psel_sb")
        P.dma('sp', 'misc', psel_sb[:], psel, writes=[m_t])
        xB = Ring(P, 1, [128, D], F32, name="xB")
        yA = Ring(P, 2, [128, 512], BF16, name="yA")
        yB = Ring(P, 2, [128, 512], BF16, name="yB")
    Woutb = P.sb([128, 8, D], BF16, "Woutb"); Wout_t = Trk()
    for c in range(8):
        P.dma('pool', 'wout', Woutb[:, c, :], Wout[c * 128:(c + 1) * 128, :], writes=[Wout_t])
    H = P.sb([128, nb, D], F32, "H")
    H_t = [Trk() for _ in range(nb)]
    u2T = P.sb([128, 8, TB], BF16, "u2T")
    u2T_t = [Trk() for _ in range(nb)]
    yTb = Ring(P, 1, [128, 8, 512], BF16, name="yTb")
    udt = F32 if moe else BF16
    u = Ring(P, 1 if moe else 2, [128, D], udt, name="u")
    junk = P.sb([128, D], BF16, "junk"); junk_t = Trk()
    ss = Ring(P, 4, [128, 1], F32, name="ss")
    pt = Ring(P, 1 if moe else 2, [128, D], udt, psum=True, name="pt")
    psA = Ring(P, 2, [128, 512], F32, psum=True, name="psA")
    psg = Ring(P, 2, [128, 512], F32, psum=True, name="psg")
    psu = Ring(P, 2, [128, 512], F32, psum=True, name="psu")
    wg = Ring(P, 3, [128, 8, FG], BF16, name="wg")
    wu = Ring(P, 3, [128, 8, FG], BF16, name="wu")
    wd = Ring(P, 3, [128, nj, D], BF16, name="wd")
    actT = Ring(P, 2, [128, nj, TB], BF16, name="actT")
    stmp = Ring(P, 2, [128, 512], F32, name="stmp")
    if moe:
        uTf = Ring(P, 1, [128, 8, 128], F32, name="uTf")
        comb = P.sb([128, nb, 8], F32, "comb"); comb_t = [Trk() for _ in range(nb)]
        lg = Ring(P, 2, [128, 8], F32, name="lg")
        mx = Ring(P, 2, [128, 8], F32, name="mx")
        gt = Ring(P, 2, [128, 4], F32, name="gt")
    ident = C['idf'] if moe else C['idb']
    out_t = Trk()
    nev = 0
    for ps_ in range(npass):
        t0 = ps_ * TB
        for sub in range(nsub):
            yb, yb_t = yTb.next()
            if sel:
                for c in range(8):
                    ya, ya_t = yA.next(); yb2, yb2_t = yB.next()
                    sl = (yA.k - 1) % 2
                    cA = t0 + sub * 512
                    P.dma('pool', f"yA{sl}", ya[:], yT[c * 128:(c + 1) * 128, cA:cA + 512], writes=[ya_t])
                    P.dma('pool', f"yB{sl}", yb2[:], yT[c * 128:(c + 1) * 128, T + cA:T + cA + 512], writes=[yb2_t])
                    _ts(P, 'dve', ya[:], ya[:], psel_sb[:, 0:1], None, ALU.mult, None, [ya_t, m_t], [ya_t])
                    _stt(P, 'dve', yb[:, c, :], yb2[:], psel_sb[:, 1:2], ya[:], ALU.mult, ALU.add, [yb2_t, ya_t, m_t], [yb_t])
            else:
                P.dma('pool', 'ytb', yb[:], yT[:, t0 + sub * 512:t0 + (sub + 1) * 512].rearrange("(c p) t -> p c t", p=128), writes=[yb_t])
            for blk in range(4):
                b = sub * 4 + blk
                r0 = t0 + b * 128
                P.dma('sp', f"xld{b % 4}", H[:, b, :], x[r0:r0 + 128, :], writes=[H_t[b]])
                if sel:
                    xb_, xb_t = xB.next()
                    P.dma('sp', "xldB", xb_[:], x[T + r0:T + r0 + 128, :], writes=[xb_t])
                    _ts(P, 'dve', H[:, b, :], H[:, b, :], psel_sb[:, 0:1], None, ALU.mult, None, [H_t[b], m_t], [H_t[b]])
                    _stt(P, 'dve', H[:, b, :], xb_[:], psel_sb[:, 1:2], H[:, b, :], ALU.mult, ALU.add, [xb_t, H_t[b], m_t], [H_t[b]])
                for half in range(2):
                    ps, ps_t = psA.next()
                    for c in range(8):
                        _mm(P, ps[:], yb[:, c, blk * 128:(blk + 1) * 128], Woutb[:, c, half * 512:(half + 1) * 512], c == 0, c == 7,
                            [yb_t, Wout_t], [ps_t])
                    _tt(P, 'dve', H[:, b, half * 512:(half + 1) * 512], ps[:], H[:, b, half * 512:(half + 1) * 512], ALU.add,
                        [ps_t, H_t[b]], [H_t[b]])
                s_, s_t = ss.next()
                _act(P, junk[:], H[:, b, :], AF.Square, [H_t[b]], [junk_t, s_t], accum=s_[:])
                _ts(P, 'dve', s_[:], s_[:], 1.0 / D, 1e-6, ALU.mult, ALU.add, [s_t], [s_t])
                _act(P, s_[:], s_[:], AF.Sqrt, [s_t], [s_t])
                _recip(P, s_[:], s_[:], [s_t], [s_t])
                u_, u_t = u.next()
                _stt(P, 'dve', u_[:], H[:, b, :], s_[:, 0:1], gbc[:], ALU.mult, ALU.mult, [H_t[b], s_t, m_t], [u_t])
                p_, p_t = pt.next()
                for c in range(8):
                    _tr(P, p_[:, c * 128:(c + 1) * 128], u_[:, c * 128:(c + 1) * 128], ident[:], [u_t, C['trk']], [p_t])
                if moe:
                    uf, uf_t = uTf.next()
                    _cp(P, 'act', uf[:], p_[:].rearrange("p (c t) -> p c t", t=128), [p_t], [uf_t])
                    _cp(P, 'dve', u2T[:, :, b * 128:(b + 1) * 128], uf[:], [uf_t], [u2T_t[b]])
                    ps, ps_t = psA.next()
                    for c in range(8):
                        _mm(P, ps[:, 0:8], uf[:, c, :], rt_sb[:, c, :], c == 0, c == 7, [uf_t, m_t], [ps_t])
                    l_, l_t = lg.next()
                    _cp(P, 'act', l_[:], ps[:, 0:8], [ps_t], [l_t])
                    m_, mm_t = mx.next()
                    P.op('dve', lambda e, m_=m_, l_=l_: e.max(out=m_[:], in_=l_[:]), [l_t], [mm_t])
                    g_, g_t = gt.next()
                    _tt(P, 'dve', g_[:, 0:1], m_[:, 0:1], m_[:, 1:2], ALU.subtract, [mm_t], [g_t])
                    _act(P, g_[:, 1:2], g_[:, 0:1], AF.Sigmoid, [g_t], [g_t], scale=-1.0)
                    _act(P, g_[:, 0:1], g_[:, 0:1], AF.Sigmoid, [g_t], [g_t])
                    _ts(P, 'dve', comb[:, b, :], l_[:], m_[:, 0:1], g_[:, 0:1], ALU.is_equal, ALU.mult, [l_t, mm_t, g_t], [comb_t[b]])
                    _ts(P, 'dve', l_[:], l_[:], m_[:, 1:2], g_[:, 1:2], ALU.is_equal, ALU.mult, [l_t, mm_t, g_t], [l_t])
                    _tt(P, 'dve', comb[:, b, :], comb[:, b, :], l_[:], ALU.add, [l_t, comb_t[b]], [comb_t[b]])
                else:
                    eng = 'dve' if nev % 2 == 0 else 'act'
                    nev += 1
                    _cp(P, eng, u2T[:, :, b * 128:(b + 1) * 128], p_[:].rearrange("p (c t) -> p c t", t=128), [p_t], [u2T_t[b]])
        for e in range(NE):
            for fg in range(ngrp):
                f0 = fg * FG
                wg_, wg_t = wg.next(); wu_, wu_t = wu.next(); wd_, wd_t = wd.next()
                sl = (wg.k - 1) % 3
                P.dma('pool', f"wg{sl}", wg_[:], Wg[e, :, f0:f0 + FG].rearrange("(c p) f -> p c f", p=128), writes=[wg_t])
                P.dma('pool', f"wu{sl}", wu_[:], Wu[e, :, f0:f0 + FG].rearrange("(c p) f -> p c f", p=128), writes=[wu_t])
                P.dma('pool', f"wd{sl}", wd_[:], Wd[e, f0:f0 + FG, :].rearrange("(j p) d -> p j d", p=128), writes=[wd_t])
                a_, a_t = actT.next()
                for j in range(nj):
                    for n in range(TB // 512):
                        ut = u2T_t[n * 4:(n + 1) * 4]
                        pg, pg_t = psg.next()
                        for c in range(8):
                            _mm(P, pg[:], wg_[:, c, j * 128:(j + 1) * 128], u2T[:, c, n * 512:(n + 1) * 512], c == 0, c == 7, [wg_t] + ut, [pg_t])
                        pu, pu_t = psu.next()
                        for c in range(8):
                            _mm(P, pu[:], wu_[:, c, j * 128:(j + 1) * 128], u2T[:, c, n * 512:(n + 1) * 512], c == 0, c == 7, [wu_t] + ut, [pu_t])
                        st_, st_t = stmp.next()
                        _act(P, st_[:], pg[:], AF.Silu, [pg_t], [st_t])
                        _tt(P, 'dve', a_[:, j, n * 512:(n + 1) * 512], st_[:], pu[:], ALU.mult, [st_t, pu_t], [a_t])
                for b in range(nb):
                    for half in range(2):
                        ps, ps_t = psA.next()
                        for j in range(nj):
                            _mm(P, ps[:], a_[:, j, b * 128:(b + 1) * 128], wd_[:, j, half * 512:(half + 1) * 512], j == 0, j == nj - 1,
                                [a_t, wd_t], [ps_t])
                        hs = H[:, b, half * 512:(half + 1) * 512]
                        if moe:
                            _stt(P, 'dve', hs, ps[:], comb[:, b, e:e + 1], hs, ALU.mult, ALU.add, [ps_t, comb_t[b], H_t[b]], [H_t[b]])
                        else:
                            _tt(P, 'dve', hs, ps[:], hs, ALU.add, [ps_t, H_t[b]], [H_t[b]])
        for b in range(nb):
            r0 = t0 + b * 128
            if moe:
                s_, s_t = ss.next()
                _act(P, junk[:], H[:, b, :], AF.Square, [H_t[b]], [junk_t, s_t], accum=s_[:])
                _ts(P, 'dve', s_[:], s_[:], 1.0 / D, 1e-6, ALU.mult, ALU.add, [s_t], [s_t])
                _act(P, s_[:], s_[:], AF.Sqrt, [s_t], [s_t])
                _recip(P, s_[:], s_[:], [s_t], [s_t])
                _stt(P, 'dve', H[:, b, :], H[:, b, :], s_[:, 0:1], fgbc[:], ALU.mult, ALU.mult, [H_t[b], s_t, m_t], [H_t[b]])
            P.dma('sp', 'ost', out[r0:r0 + 128, :], H[:, b, :], reads=[H_t[b]], writes=[out_t])
    return env.finish([out_t], 'sp')


def build_rwkv(S, NT=512, env=None):
    env = env or Env()
    nc = env.nc
    x = env.dram("x", [S, D_MODEL], F32, "ExternalInput")
    gain = env.dram("gain", [128, 8], F32, "ExternalInput")
    W = env.dram("W", [D_MODEL, 1024], F32, "ExternalInput")
    mu = env.dram("mu", [128, 8], F32, "ExternalInput")
    prm = env.dram("prm", [128, 2, 8], F32, "ExternalInput")
    w2 = env.dram("w2", [64, 256], F32, "ExternalInput")
    a2 = env.dram("a2", [64, 256], F32, "ExternalInput")
    g2 = env.dram("g2", [128, 256], F32, "ExternalInput")
    yT = env.dram("yT", [256, S], F32, "ExternalOutput")
    P = env.P
    C = env.consts()
    idf = C['idf']
    NC = NT // 64
    ntile = S // NT
    m_t = Trk()
    g_sb = P.sb([128, 8], F32, "gain_sb")
    P.dma('sp', 'misc', g_sb[:], gain, writes=[m_t])
    mu_sb = P.sb([128, 8], F32, "mu_sb")
    P.dma('sp', 'misc', mu_sb[:], mu, writes=[m_t])
    prm_sb = P.sb([128, 2, 8], F32, "prm_sb")
    P.dma('sp', 'misc', prm_sb[:], prm, writes=[m_t])
    w2_sb = P.sb([64, 256], F32, "w2_sb")
    P.dma('sp', 'misc', w2_sb[:], w2, writes=[m_t])
    a2_sb = P.sb([128, 256], F32, "a2_sb")
    P.dma('sp', 'misc', a2_sb[64:128, :], a2, writes=[m_t])
    g2_sb = P.sb([128, 256], F32, "g2_sb")
    P.dma('sp', 'misc', g2_sb[:], g2, writes=[m_t])
    _ts(P, 'dve', prm_sb[:, :, 7:8], prm_sb[:, :, 3:4], -1.0, 1.0, ALU.mult, ALU.add, [m_t], [m_t])
    Wb, W_t = load_w(P, W, D_MODEL, 1024, g_sb, m_t, "Wb")
    fe = Front(P, C, x, NT, n_uT=1, n_xs=2)
    PS = Ring(P, 5, [128, 512], F32, psum=True, name="PS")
    PSB = Ring(P, 1, [128, 1024], BF16, psum=True, name="PSB")
    k_t = Trk()
    bones = P.sb([128, 128], F32, "bones")
    _memset(P, 'pool', bones[:], 0.0, [k_t])
    _memset(P, 'pool', bones[0:64, 0:64], 1.0, [k_t])
    _memset(P, 'pool', bones[64:128, 64:128], 1.0, [k_t])
    smask = P.sb([128, NT], F32, "smask")
    _memset(P, 'pool', smask[:], 1.0, [k_t])
    _memset(P, 'pool', smask[:, 0:NT:64], 0.0, [k_t])
    mus = P.sb([64, 64], F32, "mus"); mui = P.sb([64, 64], F32, "mui"); mls = P.sb([64, 64], F32, "mls")
    for (t_, base, cm, pat) in ((mus, -1, -1, 1), (mui, 0, -1, 1), (mls, -1, 1, -1)):
        _memset(P, 'pool', t_[:], 1.0, [k_t])
        P.op('pool', lambda e, t_=t_, base=base, cm=cm, pat=pat: e.affine_select(
            out=t_[:], in_=t_[:], pattern=[[pat, 64]], compare_op=ALU.is_ge, fill=0.0, base=base, channel_multiplier=cm), [k_t], [k_t])
    maskA = P.sb([64, 2, 256], F32, "maskA")
    for j in range(2):
        for q_, src in enumerate((mus, mui, mus, mui)):
            _cp(P, 'pool', maskA[:, j, q_ * 64:(q_ + 1) * 64], src[:], [k_t], [k_t])
    maskP = P.sb([64, 8, 64], F32, "maskP")
    for j in range(8):
        _cp(P, 'pool', maskP[:, j, :], mls[:], [k_t], [k_t])
    id8 = P.sb([64, 8, 64], F32, "id8")
    for j in range(8):
        _cp(P, 'pool', id8[:, j, :], idf[0:64, 0:64], [C['trk']], [k_t])
    sh = [P.sb([128, NT + 1], F32, f"sh{c}") for c in range(8)]
    sh_t = [Trk() for _ in range(8)]
    for c in range(8):
        _memset(P, 'pool', sh[c][:, 0:1], 0.0, [sh_t[c]])
    X = [P.sb([128, NT], F32, f"X{c}") for c in range(8)]
    X_t = [Trk() for _ in range(8)]
    Hs = [P.sb([128, 64], F32, f"Hs{hp}") for hp in range(2)]
    Hs_t = [Trk() for _ in range(2)]
    for hp in range(2):
        _memset(P, 'pool', Hs[hp][:], 0.0, [Hs_t[hp]])
    names = ["th", "sgd", "lw", "a", "g", "kk", "kp", "bb", "L", "eL", "eLm", "eLp", "eC", "t1", "t2", "BT", "KTt", "KH", "BH",
             "bonus", "YT"]
    LP = BF16
    E = {n: P.sb([128, NT], LP if n in ("BT", "KTt", "KH", "BH") else F32, "e_" + n) for n in names}
    vb = P.sb([128, NT], LP, "vb"); vb_t = Trk()
    Hb = [P.sb([128, 64], LP, f"Hb{hp}") for hp in range(2)]
    Hb_t = [Trk() for _ in range(2)]
    for hp in range(2):
        _memset(P, 'pool', Hb[hp][:], 0.0, [Hb_t[hp]])
    Et = {n: Trk() for n in names}
    AR = P.sb([128, NC, 2, 64], LP, "AR"); AR_t = Trk()
    gC = P.sb([128, NC], F32, "gC"); gC_t = Trk()
    TM = {n: P.sb([64, NC, 128], LP, "tm_" + n) for n in ("V", "A", "KH", "BH")}
    TM_t = {n: Trk() for n in TM}
    M12 = [P.sb([64, NC, 256], LP, f"M12_{h}") for h in range(2)]; M12_t = [Trk(), Trk()]
    PQ = [[P.sb([64, NC, 128], LP, f"PQ{h}_{j}") for j in range(2)] for h in range(2)]; PQ_t = [[Trk(), Trk()], [Trk(), Trk()]]
    TT = [P.sb([64, NC, 64], LP, f"TT{h}") for h in range(2)]; TT_t = [Trk(), Trk()]
    Zs = [P.sb([64, NC, 64], LP, f"Zs{h}") for h in range(2)]; Zs_t = [Trk(), Trk()]
    W1T = P.sb([128, NC, 64], LP, "W1T"); W1T_t = Trk()
    U0s = P.sb([64, NC, 2, 64], F32, "U0s"); U0s_t = Trk()
    M12b = P.sb([64, 2, NC, 128], LP, "M12b"); M12b_t = Trk()
    Us = Ring(P, 2, [64, 128], LP, name="Us")
    Ysb = P.sb([64, NC, 128], F32, "Ysb"); Ysb_t = Trk()
    Ysq = P.sb([64, NC, 128], F32, "Ysq"); Ysq_t = Trk()
    st = P.sb([64, 4, NC * 2], F32, "gnst"); st_t = Trk()
    out_t = Trk()
    CE = -float(np.exp(-0.5))

    for i in range(ntile):
        uT, uT_t = fe.tile(i)
        for c in range(8):
            ps, ps_t = PS.next()
            proj_fm(P, ps, ps_t, Wb, W_t, c * 128, 128, uT, uT_t, NT)
            _cp(P, 'act', sh[c][:, 1:NT + 1], ps[:, 0:NT], [ps_t], [sh_t[c]])
            _tt(P, 'dve', E["t1"][:], sh[c][:, 0:NT], sh[c][:, 1:NT + 1], ALU.subtract, [sh_t[c]], [Et["t1"]])
            _stt(P, 'dve', X[c][:], E["t1"][:], mu_sb[:, c:c + 1], sh[c][:, 1:NT + 1], ALU.mult, ALU.add, [Et["t1"], sh_t[c], m_t], [X_t[c]])
            _cp(P, 'act', sh[c][:, 0:1], sh[c][:, NT:NT + 1], [sh_t[c]], [sh_t[c]])
        if STOP == 21: break
        _act(P, E["th"][0:64, :], X[6][0:64, :], AF.Tanh, [X_t[6]], [Et["th"]])
        _act(P, E["sgd"][:], X[7][:], AF.Sigmoid, [X_t[7]], [Et["sgd"]])
        for hp in range(2):
            r, k, v = X[hp], X[2 + hp], X[4 + hp]
            r_t, k_tk, v_t = X_t[hp], X_t[2 + hp], X_t[4 + hp]
            pr = lambda j: prm_sb[:, hp, j:j + 1]
            hc = slice(hp * 128, (hp + 1) * 128)
            ps, ps_t = PS.next()
            _mm(P, ps[:, 0:NT], w2_sb[0:64, hc], E["th"][0:64, :], True, True, [m_t, Et["th"]], [ps_t])
            _act(P, E["lw"][:], ps[:, 0:NT], AF.Sigmoid, [ps_t, m_t], [Et["lw"]], bias=pr(0))
            _ts(P, 'dve', E["lw"][:], E["lw"][:], CE, None, ALU.mult, None, [Et["lw"]], [Et["lw"]])
            P.op('dve', lambda e: e.tensor_tensor_scan(out=E["L"][:], data0=smask[:], data1=E["lw"][:], initial=0.0,
                                                       op0=ALU.mult, op1=ALU.add), [k_t, Et["lw"]], [Et["L"]])
            ps, ps_t = PS.next()
            _mm(P, ps[:, 0:NT], a2_sb[64:128, hc], X[6][64:128, :], True, True, [m_t, X_t[6]], [ps_t])
            _act(P, E["a"][:], ps[:, 0:NT], AF.Sigmoid, [ps_t, m_t], [Et["a"]], bias=pr(1))
            ps, ps_t = PS.next()
            _mm(P, ps[:, 0:NT], g2_sb[:, hc], E["sgd"][:], True, True, [m_t, Et["sgd"]], [ps_t])
            _cp(P, 'act', E["g"][:], ps[:, 0:NT], [ps_t], [Et["g"]])
            _ts(P, 'dve', E["kk"][:], k[:], pr(2), None, ALU.mult, None, [k_tk, m_t], [Et["kk"]])
            _act(P, E["t1"][:], E["kk"][:], AF.Square, [Et["kk"]], [Et["t1"]])
            ps, ps_t = PS.next()
            _mm(P, ps[:, 0:NT], bones[:], E["t1"][:], True, True, [k_t, Et["t1"]], [ps_t])
            _act(P, E["t2"][:], ps[:, 0:NT], AF.Sqrt, [ps_t], [Et["t2"]])
            _ts(P, 'dve', E["t2"][:], E["t2"][:], 1e-12, None, ALU.max, None, [Et["t2"]], [Et["t2"]])
            _recip(P, E["t2"][:], E["t2"][:], [Et["t2"]], [Et["t2"]])
            _tt(P, 'pool', E["kk"][:], E["kk"][:], E["t2"][:], ALU.mult, [Et["kk"], Et["t2"]], [Et["kk"]])
            _ts(P, 'dve', E["t1"][:], E["a"][:], pr(3), pr(7), ALU.mult, ALU.add, [Et["a"], m_t], [Et["t1"]])
            _tt(P, 'pool', E["kp"][:], k[:], E["t1"][:], ALU.mult, [k_tk, Et["t1"]], [Et["kp"]])
            _tt(P, 'pool', E["bb"][:], E["kk"][:], E["a"][:], ALU.mult, [Et["kk"], Et["a"]], [Et["bb"]])
            L3 = E["L"][:].rearrange("p (c t) -> p c t", t=64)
            _act(P, E["eL"][:], E["L"][:], AF.Exp, [Et["L"]], [Et["eL"]])
            _act(P, E["eLm"][:], E["L"][:], AF.Exp, [Et["L"]], [Et["eLm"]], scale=-1.0)
            _tt(P, 'dve', E["t2"][:], E["L"][:], E["lw"][:], ALU.subtract, [Et["L"], Et["lw"]], [Et["t2"]])
            _act(P, E["eLp"][:], E["t2"][:], AF.Exp, [Et["t2"]], [Et["eLp"]])
            _tt(P, 'dve', E["t1"][:].rearrange("p (c t) -> p c t", t=64), L3[:, :, 63:64].to_broadcast([128, NC, 64]), L3,
                ALU.subtract, [Et["L"]], [Et["t1"]])
            _act(P, E["eC"][:], E["t1"][:], AF.Exp, [Et["t1"]], [Et["eC"]])
            _act(P, gC[:], E["L"][:, 63:NT:64], AF.Exp, [Et["L"]], [gC_t])
            if STOP == 22: break
            v3 = lambda t: t[:].rearrange("p (c t) -> p c t", t=64)
            _stt(P, 'dve', AR[:, :, 0, :], v3(E["kk"]), -1.0, v3(E["eLp"]), ALU.mult, ALU.mult, [Et["kk"], Et["eLp"]], [AR_t])
            _tt(P, 'pool', AR[:, :, 1, :], v3(r), v3(E["eL"]), ALU.mult, [r_t, Et["eL"]], [AR_t])
            _tt(P, 'dve', E["BT"][:], E["bb"][:], E["eLm"][:], ALU.mult, [Et["bb"], Et["eLm"]], [Et["BT"]])
            _tt(P, 'pool', E["KTt"][:], E["kp"][:], E["eLm"][:], ALU.mult, [Et["kp"], Et["eLm"]], [Et["KTt"]])
            _tt(P, 'dve', E["KH"][:], E["kp"][:], E["eC"][:], ALU.mult, [Et["kp"], Et["eC"]], [Et["KH"]])
            _tt(P, 'pool', E["BH"][:], E["bb"][:], E["eC"][:], ALU.mult, [Et["bb"], Et["eC"]], [Et["BH"]])
            _stt(P, 'dve', E["t1"][:], r[:], pr(4), E["kp"][:], ALU.mult, ALU.mult, [r_t, Et["kp"], m_t], [Et["t1"]])
            ps, ps_t = PS.next()
            _mm(P, ps[:, 0:NT], bones[:], E["t1"][:], True, True, [k_t, Et["t1"]], [ps_t])
            _tt(P, 'dve', E["bonus"][:], ps[:, 0:NT], v[:], ALU.mult, [ps_t, v_t], [Et["bonus"]])
            if STOP == 23: break
            _cp(P, 'act', vb[:], v[:], [v_t], [vb_t])
            for qi, (n, src, src_t) in enumerate((("V", vb, vb_t), ("A", None, AR_t), ("KH", E["KH"], Et["KH"]), ("BH", E["BH"], Et["BH"]))):
                ps, ps_t = PSB.next()
                for c in range(NC):
                    in_ = AR[:, c, 0, :] if src is None else src[:, c * 64:(c + 1) * 64]
                    _tr(P, ps[0:64, c * 128:(c + 1) * 128], in_, C['idb'][:], [src_t, C['trk']], [ps_t])
                _cp(P, 'act' if qi % 2 else 'dve', TM[n][:], ps[0:64, 0:NC * 128].rearrange("p (c t) -> p c t", t=128), [ps_t], [TM_t[n]])
            if STOP == 24: break
            AR2 = AR[:].rearrange("p c a t -> p c (a t)")
            HS = [slice(0, 64), slice(64, 128)]
            for hh in range(2):
                hs = HS[hh]
                for pr2 in range(NC // 2):
                    ps, ps_t = PS.next()
                    for cc in range(2):
                        c = pr2 * 2 + cc
                        _mm(P, ps[0:64, cc * 256:cc * 256 + 128], E["BT"][hs, c * 64:(c + 1) * 64], AR2[hs, c, :], True, True,
                            [Et["BT"], AR_t], [ps_t])
                        _mm(P, ps[0:64, cc * 256 + 128:cc * 256 + 256], E["KTt"][hs, c * 64:(c + 1) * 64], AR2[hs, c, :], True, True,
                            [Et["KTt"], AR_t], [ps_t])
                    _tt(P, 'dve', M12[hh][:, pr2 * 2:pr2 * 2 + 2, :], ps[0:64, :].rearrange("p (c t) -> p c t", t=256), maskA[:],
                        ALU.mult, [ps_t, k_t], [M12_t[hh]])
            for hh in range(2):
                hs = HS[hh]
                ps, ps_t = PS.next()
                for c in range(NC):
                    _mm(P, ps[0:64, c * 64:(c + 1) * 64], AR[hs, c, 0, :], E["BT"][hs, c * 64:(c + 1) * 64], True, True, [AR_t, Et["BT"]], [ps_t])
                _tt(P, 'dve', PQ[hh][0][:, :, 0:64], ps[0:64, 0:NC * 64].rearrange("p (c t) -> p c t", t=64), maskP[:, 0:NC, :], ALU.mult,
                    [ps_t, k_t], [PQ_t[hh][0]])
                _cp(P, 'pool', PQ[hh][0][:, :, 64:128], M12[hh][:, :, 0:64], [M12_t[hh]], [PQ_t[hh][0]])
                _tt(P, 'pool', TT[hh][:], M12[hh][:, :, 0:64], id8[:, 0:NC, :], ALU.add, [M12_t[hh], k_t], [TT_t[hh]])
                _cp(P, 'act', M12b[:, hh, :, 0:64], M12[hh][:, :, 64:128], [M12_t[hh]], [M12b_t])
                _cp(P, 'act', M12b[:, hh, :, 64:128], M12[hh][:, :, 192:256], [M12_t[hh]], [M12b_t])
            cur = 0
            for lvl in range(5):
                nxt = 1 - cur
                last = lvl == 4
                for hh in range(2):
                    for half in range(NC // 4):
                        ps, ps_t = PS.next()
                        for cc in range(4):
                            c = half * 4 + cc
                            _mm(P, ps[0:64, cc * 128:cc * 128 + 64], PQ[hh][cur][:, c, 64:128], PQ[hh][cur][:, c, 0:64], True, True,
                                [PQ_t[hh][cur]], [ps_t])
                            if not last:
                                _mm(P, ps[0:64, cc * 128 + 64:cc * 128 + 128], PQ[hh][cur][:, c, 0:64], PQ[hh][cur][:, c, 64:128], True, True,
                                    [PQ_t[hh][cur]], [ps_t])
                        if last:
                            _cp(P, 'act', PQ[hh][nxt][:, half * 4:(half + 1) * 4, 0:64],
                                ps[0:64, :].rearrange("p (c t) -> p c t", t=128)[:, :, 0:64], [ps_t], [PQ_t[hh][nxt]])
                        else:
                            _cp(P, 'act' if (half + hh) % 2 else 'dve', PQ[hh][nxt][:, half * 4:(half + 1) * 4, :],
                                ps[0:64, :].rearrange("p (c t) -> p c t", t=128), [ps_t], [PQ_t[hh][nxt]])
                for hh in range(2):
                    ps, ps_t = PS.next()
                    for c in range(NC):
                        _mm(P, ps[0:64, c * 64:(c + 1) * 64], PQ[hh][nxt][:, c, 0:64], TT[hh][:, c, :], True, True,
                            [PQ_t[hh][nxt], TT_t[hh]], [ps_t])
                    _tt(P, 'dve', TT[hh][:], ps[0:64, 0:NC * 64].rearrange("p (c t) -> p c t", t=64), TT[hh][:], ALU.add,
                        [ps_t, TT_t[hh]], [TT_t[hh]])
                cur = nxt
            for hh in range(2):
                vc = HS[hh]
                ps, ps_t = PS.next()
                for c in range(NC):
                    _mm(P, ps[0:64, c * 64:(c + 1) * 64], M12[hh][:, c, 128:192], TM["V"][:, c, vc], True, True, [M12_t[hh], TM_t["V"]], [ps_t])
                _cp(P, 'act', Zs[hh][:], ps[0:64, 0:NC * 64].rearrange("p (c t) -> p c t", t=64), [ps_t], [Zs_t[hh]])
            for hh in range(2):
                hs = HS[hh]
                ps, ps_t = PS.next()
                for c in range(NC):
                    _mm(P, ps[:, c * 64:(c + 1) * 64], TM["A"][:, c, :], TT[hh][:, c, :], True, True, [TM_t["A"], TT_t[hh]], [ps_t])
                _cp(P, 'dve', W1T[hs, :, :], ps[hs, 0:NC * 64].rearrange("p (c t) -> p c t", t=64), [ps_t], [W1T_t])
            for hh in range(2):
                ps, ps_t = PS.next()
                for c in range(NC):
                    _mm(P, ps[0:64, c * 64:(c + 1) * 64], TT[hh][:, c, :], Zs[hh][:, c, :], True, True, [TT_t[hh], Zs_t[hh]], [ps_t])
                _cp(P, 'act', U0s[:, :, hh, :], ps[0:64, 0:NC * 64].rearrange("p (c t) -> p c t", t=64), [ps_t], [U0s_t])
            if STOP in (25, 26, 27): break
            for c in range(NC):
                ps, ps_t = PS.next()
                for hh in range(2):
                    hs = slice(hh * 64, (hh + 1) * 64)
                    _mm(P, ps[0:64, hh * 64:(hh + 1) * 64], W1T[hs, c, :], Hb[hp][hs, :], True, True, [W1T_t, Hb_t[hp]], [ps_t], force_own=(hh == 1))
                us, us_t = Us.next()
                _tt(P, 'dve', us[:], ps[0:64, 0:128], U0s[:, c, :, :].rearrange("p a t -> p (a t)"), ALU.add, [ps_t, U0s_t], [us_t])
                psy, psy_t = PS.next()
                for hh in range(2):
                    hs = slice(hh * 64, (hh + 1) * 64)
                    vc = slice(hh * 64, (hh + 1) * 64)
                    _mm(P, psy[0:64, vc], AR[hs, c, 1, :], Hb[hp][hs, :], True, False, [AR_t, Hb_t[hp]], [psy_t], force_own=(hh == 1))
                    _mm(P, psy[0:64, vc], M12b[:, hh, c, 0:64], us[:, vc], False, False, [M12b_t, us_t], [psy_t], force_own=(hh == 1))
                    _mm(P, psy[0:64, vc], M12b[:, hh, c, 64:128], TM["V"][:, c, vc], False, True, [M12b_t, TM_t["V"]], [psy_t])
                _cp(P, 'act', Ysb[:, c, :], psy[0:64, 0:128], [psy_t], [Ysb_t])
                psh, psh_t = PS.next()
                _mm(P, psh[:, 0:128], TM["KH"][:, c, :], TM["V"][:, c, :], True, False, [TM_t["KH"], TM_t["V"]], [psh_t])
                _mm(P, psh[:, 0:128], TM["BH"][:, c, :], us[:], False, True, [TM_t["BH"], us_t], [psh_t])
                for hh in range(2):
                    hs = slice(hh * 64, (hh + 1) * 64)
                    _stt(P, 'dve', Hs[hp][hs, :], Hs[hp][hs, :], gC[hs, c:c + 1], psh[hs, hh * 64:(hh + 1) * 64], ALU.mult, ALU.add,
                         [Hs_t[hp], gC_t, psh_t], [Hs_t[hp]])
                _cp(P, 'act', Hb[hp][:], Hs[hp][:], [Hs_t[hp]], [Hb_t[hp]])
            if STOP == 28: break
            Y4 = Ysb[:].rearrange("p c (a t) -> p (c a) t", t=64)
            P.op('dve', lambda e: e.tensor_reduce(out=st[:, 0, :], in_=Ysb[:].rearrange("p c (a t) -> p (c a) t", t=64), axis=AX.X, op=ALU.add),
                 [Ysb_t], [st_t])
            _act(P, Ysq[:], Ysb[:], AF.Square, [Ysb_t], [Ysq_t])
            P.op('dve', lambda e: e.tensor_reduce(out=st[:, 1, :], in_=Ysq[:].rearrange("p c (a t) -> p (c a) t", t=64), axis=AX.X, op=ALU.add),
                 [Ysq_t], [st_t])
            _ts(P, 'dve', st[:, 0, :], st[:, 0, :], 1.0 / 64, None, ALU.mult, None, [st_t], [st_t])
            _tt(P, 'dve', st[:, 2, :], st[:, 0, :], st[:, 0, :], ALU.mult, [st_t], [st_t])
            _stt(P, 'dve', st[:, 1, :], st[:, 1, :], 1.0 / 64, st[:, 2, :], ALU.mult, ALU.subtract, [st_t], [st_t])
            _ts(P, 'dve', st[:, 1, :], st[:, 1, :], 64e-5, None, ALU.add, None, [st_t], [st_t])
            _act(P, st[:, 1, :], st[:, 1, :], AF.Sqrt, [st_t], [st_t])
            _recip(P, st[:, 1, :], st[:, 1, :], [st_t], [st_t])
            Yq4 = Ysq[:].rearrange("p c (a t) -> p (c a) t", t=64)
            _tt(P, 'dve', Yq4, Y4, st[:, 0, :].unsqueeze(2).to_broadcast([64, NC * 2, 64]), ALU.subtract, [Ysb_t, st_t], [Ysq_t])
            _tt(P, 'dve', Yq4, Yq4, st[:, 1, :].unsqueeze(2).to_broadcast([64, NC * 2, 64]), ALU.mult, [Ysq_t, st_t], [Ysq_t])
            ps, ps_t = PS.next()
            for c in range(NC):
                _tr(P, ps[:, c * 64:(c + 1) * 64], Ysq[:, c, :], idf[0:64, 0:64], [Ysq_t, C['trk']], [ps_t])
            _ts(P, 'dve', E["YT"][:], ps[:, 0:NT], pr(5), pr(6), ALU.mult, ALU.add, [ps_t, m_t], [Et["YT"]])
            _tt(P, 'pool', E["YT"][:], E["YT"][:], E["bonus"][:], ALU.add, [Et["YT"], Et["bonus"]], [Et["YT"]])
            _tt(P, 'pool', E["YT"][:], E["YT"][:], E["g"][:], ALU.mult, [Et["YT"], Et["g"]], [Et["YT"]])
            P.dma('sp', 'yst', yT[hp * 128:(hp + 1) * 128, i * NT:(i + 1) * NT], E["YT"][:], reads=[Et["YT"]], writes=[out_t])
    return env.finish([out_t], 'sp')


def rwkv_inputs(xb, p, norm_mix, w_in, shift_mu, w0, w2, a0, a2, g2, kk, ka, rk, lnx_g, lnx_b):
    hs = slice(p * 256, (p + 1) * 256)
    cols = np.concatenate([np.arange(0, 512)[hs], 512 + np.arange(0, 512)[hs], 1024 + np.arange(0, 512)[hs], np.arange(1536, 1792)])
    W = w_in[:, cols]
    mu = shift_mu[cols]
    prm = np.zeros((128, 2, 8), np.float32)
    for j, a in enumerate((w0, a0, kk, ka, rk.reshape(-1), lnx_g, lnx_b)):
        prm[:, :, j] = a[hs].reshape(2, 128).T
    return {"x": xb, "gain": np.ascontiguousarray(norm_mix.reshape(8, 128).T), "W": np.ascontiguousarray(W),
            "mu": np.ascontiguousarray(mu.reshape(8, 128).T), "prm": prm, "w2": np.ascontiguousarray(w2[:, hs]),
            "a2": np.ascontiguousarray(a2[:, hs]), "g2": np.ascontiguousarray(g2[:, hs])}


def build_fused(S):
    nc = bass.Bass("TRN2", target_bir_lowering=False)
    P = Prog(nc)
    T = S // 2
    x = nc.dram_tensor("x", [S, D_MODEL], F32, kind="ExternalInput").ap()
    Y0 = nc.dram_tensor("Y0", [D_MODEL, S], F32).ap()
    H1 = nc.dram_tensor("H1", [S, D_MODEL], F32).ap()
    Y1 = nc.dram_tensor("Y1", [D_MODEL, S], F32).ap()
    out = nc.dram_tensor("out", [T, D_MODEL], F32, kind="ExternalOutput").ap()
    C = make_consts(P)
    dram_t = {"Y0": Trk(), "H1": Trk(), "Y1": Trk()}
    base = (nc.sbuf_base, nc.psum_base)

    def phase(fn, prefix, mapping, *args, **kw):
        env = Env(nc, P, C, mapping, prefix)
        fn(*args, env=env, **kw)
        P.barrier()
        nc.sbuf_base, nc.psum_base = base

    for g in range(2):
        phase(build_rwkv, f"rw{g}_", {"x": x, "yT": Y0[g * 256:(g + 1) * 256, :]}, S)
    for g in range(2):
        phase(build_mla, f"ml{g}_", {"x": x, "yT": Y0[512 + g * 256:512 + (g + 1) * 256, :]}, S)
    phase(build_tok, "t0_", {"x": x, "yT": Y0, "out": H1}, S, False, 1, 2816)
    for g in range(2):
        phase(build_fox, f"fx{g}_", {"x": H1, "yT": Y1[g * 256:(g + 1) * 256, :]}, S)
    for g in range(2):
        phase(build_moba, f"mb{g}_", {"x": H1, "yT": Y1[512 + g * 256:512 + (g + 1) * 256, :]}, S)
    phase(build_tok, "t1_", {"x": H1, "yT": Y1, "out": out}, T, True, 8, 3584, sel=True)
    P.emit()
    return nc


def fox_inputs(xb, p, norm_mix, w_in, bf):
    hs = slice(p * 256, (p + 1) * 256)
    W = np.concatenate([w_in[:, 0:512][:, hs], w_in[:, 512:1024][:, hs], w_in[:, 1024:1536][:, hs],
                        w_in[:, 1536 + 4 * p:1536 + 4 * p + 4]], axis=1)
    return {"x": xb, "gain": np.ascontiguousarray(norm_mix.reshape(8, 128).T), "W": np.ascontiguousarray(W),
            "bf": np.ascontiguousarray(bf[4 * p:4 * p + 4].reshape(4, 1))}


_NC_CACHE = {}


def _prog(name, fn, *a):
    key = (name,) + a
    if key not in _NC_CACHE:
        _NC_CACHE[key] = fn(*a)
    return _NC_CACHE[key]


def _run(nc, maps):
    res = run_bass_kernel_spmd(nc, maps, core_ids=list(range(len(maps))))
    return res.results


def kernel(**inp):
    f = {k: np.ascontiguousarray(np.asarray(v, dtype=np.float32)) for k, v in inp.items()}
    x = f['x']
    B, S, D = x.shape
    T = S // 2
    cores = [(b, p) for b in range(B) for p in range(2)]
    ra = _run(_prog("rwkv", build_rwkv, S), [rwkv_inputs(x[b], p, f['norm_mix_0'], f['w_in_0'], f['shift_mu_0'], f['rw_w0_0'],
              f['rw_w2_0'], f['rw_a0_0'], f['rw_a2_0'], f['rw_g2_0'], f['rw_kk_0'], f['rw_ka_0'], f['rw_rk_0'],
              f['rw_lnx_g_0'], f['rw_lnx_b_0']) for (b, p) in cores])
    rb = _run(_prog("mla", build_mla, S), [mla_inputs(x[b], p, f['norm_mix_0'], f['w_in_0'], f['mla_qnorm_0'], f['mla_wuq_0'],
              f['mla_kvnorm_0'], f['mla_wukv_0'], S) for (b, p) in cores])
    yT = [np.concatenate([ra[2 * b]["yT"], ra[2 * b + 1]["yT"], rb[2 * b]["yT"], rb[2 * b + 1]["yT"]], axis=0) for b in range(B)]
    del ra, rb
    maps = [{"x": np.ascontiguousarray(x[b, p * T:(p + 1) * T]), "yT": np.ascontiguousarray(yT[b][:, p * T:(p + 1) * T]),
             "Wout": f['w_out_0'], "gff": f['norm_ffn_0'].reshape(1, -1), "Wg": f['ffn_wg_0'][None], "Wu": f['ffn_wu_0'][None],
             "Wd": f['ffn_wd_0'][None]} for (b, p) in cores]
    r0 = _run(_prog("tok0", build_tok, T, False, 1, f['ffn_wg_0'].shape[1]), maps)
    h1 = [np.concatenate([r0[2 * b]["out"], r0[2 * b + 1]["out"]], axis=0) for b in range(B)]
    del r0, maps, yT
    rc = _run(_prog("fox", build_fox, S), [fox_inputs(h1[b], p, f['norm_mix_1'], f['w_in_1'], f['fox_bf_1']) for (b, p) in cores])
    rd = _run(_prog("moba", build_moba, S), [moba_inputs(h1[b], p, f['norm_mix_1'], f['w_in_1'], S) for (b, p) in cores])
    yT = [np.concatenate([rc[2 * b]["yT"], rc[2 * b + 1]["yT"], rd[2 * b]["yT"], rd[2 * b + 1]["yT"]], axis=0) for b in range(B)]
    del rc, rd
    maps = [{"x": np.ascontiguousarray(h1[b][p * T:(p + 1) * T]), "yT": np.ascontiguousarray(yT[b][:, p * T:(p + 1) * T]),
             "Wout": f['w_out_1'], "gff": f['norm_ffn_1'].reshape(1, -1), "Wg": f['moe_wg_1'], "Wu": f['moe_wu_1'],
             "Wd": f['moe_wd_1'], "router": f['router_1'], "fgain": f['final_norm'].reshape(1, -1)} for (b, p) in cores]
    r1 = _run(_prog("tok1", build_tok, T, True, 8, f['moe_wg_1'].shape[2]), maps)
    out = np.stack([np.concatenate([r1[2 * b]["out"], r1[2 * b + 1]["out"]], axis=0) for b in range(B)], axis=0)
    return out.astype(np.float32)
```
